# Optimizing a Trainium2 kernel written in Bass

```python
import jax, jax.numpy as jnp
from jax import lax
import numpy as np

D_MODEL = 4096
BATCH = 2
SEQ = 8192
DEPTH = 2

GRID_W = 64
CTX_LEN = 256
HEAD_DIM = 128
GM_GROUPS = 8
GM_WIDTH = GM_GROUPS * HEAD_DIM
GM_CHUNK = 128
NA_HEADS = 8
NA_WIDTH = NA_HEADS * HEAD_DIM
NA_ROWS = 8
NA_COLS = 16
RET_HEADS = 8
RET_DK = 128
RET_DV = 256
RET_QK_WIDTH = RET_HEADS * RET_DK
RET_V_WIDTH = RET_HEADS * RET_DV
RET_CHUNK = 128
RET_DECAY_BASE = 5.0
ROPE_BASE = 10000.0
MOE_GROUPS = 4
MOE_EXPERTS_PER_GROUP = 8
MOE_EXPERTS = MOE_GROUPS * MOE_EXPERTS_PER_GROUP
MOE_TOPK = 2
MOE_HIDDEN = 512
MOE_BLOCK = 128
EPS = 1e-6
NEG_INF = -1e30

IN_COLS = (
    ("gate_a", D_MODEL), ("gate_b", D_MODEL), ("gate_c", D_MODEL),
    ("gm_u", GM_WIDTH), ("gm_v", GM_WIDTH),
    ("na_q", NA_WIDTH), ("na_k", NA_WIDTH), ("na_v", NA_WIDTH),
    ("ret_q", RET_QK_WIDTH), ("ret_k", RET_QK_WIDTH), ("ret_v", RET_V_WIDTH), ("ret_g", RET_V_WIDTH),
)
IN_TOTAL = 3 * D_MODEL + 2 * GM_WIDTH + 3 * NA_WIDTH + 2 * RET_QK_WIDTH + 2 * RET_V_WIDTH
CTX_KV_NAMES = ("na_k", "na_v", "ret_k", "ret_v")

kernel_name = "hybrid_gmlp_natten_retention_hmoe_dit"


def _col_ranges():
    ranges, off = {}, 0
    for name, width in IN_COLS:
        ranges[name] = (off, off + width)
        off += width
    return ranges


def rms_norm(x, g):
    x32 = x.astype(jnp.float32)
    y = x32 * lax.rsqrt(jnp.mean(x32 * x32, axis=-1, keepdims=True) + EPS)
    return (y * g.astype(jnp.float32)).astype(x.dtype)


def modulate(h, shift, scale):
    return h * (1 + scale) + shift


def to_heads(t, n_heads):
    b, n, w = t.shape
    return t.reshape(b, n, n_heads, w // n_heads).transpose(0, 2, 1, 3)


def from_heads(t):
    b, h, n, d = t.shape
    return t.transpose(0, 2, 1, 3).reshape(b, n, h * d)


def chunk_gmlp(u, v, norm_g, ws, bs):
    b, n, _ = u.shape
    u = jax.nn.gelu(u)
    v32 = jax.nn.gelu(v).astype(jnp.float32)
    mu = jnp.mean(v32, axis=-1, keepdims=True)
    var = jnp.mean(jnp.square(v32 - mu), axis=-1, keepdims=True)
    vn = ((v32 - mu) * lax.rsqrt(var + EPS) * norm_g.astype(jnp.float32)).astype(u.dtype)
    vn = vn.reshape(b, n // GM_CHUNK, GM_CHUNK, GM_GROUPS, HEAD_DIM)
    mixed = jnp.einsum('gts,bnsgd->bntgd', ws, vn) + bs.T[None, None, :, :, None]
    return u * mixed.reshape(b, n, GM_WIDTH)


def neighbourhood_attention(q, k, v, k_ctx, v_ctx, rpb):
    b, h, s, hd = q.shape
    rows = s // GRID_W
    kr = min(NA_ROWS, rows)
    grid = lambda t: t.reshape(b, h, rows, GRID_W, hd)
    qg = grid(q * (hd ** -0.5))
    r = jnp.arange(rows)
    r_start = jnp.clip(r - kr // 2, 0, rows - kr)
    ridx = r_start[:, None] + jnp.arange(kr)
    cq = jnp.arange(GRID_W)
    c_start = jnp.clip(cq - NA_COLS // 2, 0, GRID_W - NA_COLS)
    col_ok = (cq[None, :] >= c_start[:, None]) & (cq[None, :] < c_start[:, None] + NA_COLS)
    dr = ridx - r[:, None] + NA_ROWS - 1
    dc = jnp.clip(cq[None, :] - cq[:, None] + NA_COLS - 1, 0, 2 * NA_COLS - 2)
    bias = rpb[:, dr[:, None, :, None], dc[None, :, None, :]].astype(jnp.float32)
    kg = grid(k)[:, :, ridx]
    vg = grid(v)[:, :, ridx]
    s_loc = jnp.einsum('bhrcd,bhrjwd->bhrcjw', qg, kg).astype(jnp.float32) + bias
    s_loc = jnp.where(col_ok[:, None, :], s_loc, NEG_INF)
    s_ctx = jnp.einsum('bhrcd,bhld->bhrcl', qg, k_ctx).astype(jnp.float32)
    n_loc = kr * GRID_W
    p = jax.nn.softmax(jnp.concatenate([s_loc.reshape(b, h, rows, GRID_W, n_loc), s_ctx], axis=-1), axis=-1)
    p = p.astype(v.dtype)
    p_loc = p[..., :n_loc].reshape(b, h, rows, GRID_W, kr, GRID_W)
    p_ctx = p[..., n_loc:]
    o = jnp.einsum('bhrcjw,bhrjwd->bhrcd', p_loc, vg) + jnp.einsum('bhrcl,bhld->bhrcd', p_ctx, v_ctx)
    return o.reshape(b, h, s, hd)


def context_attention(q, k, v):
    hd = q.shape[-1]
    sc = jnp.einsum('bhid,bhjd->bhij', q * (hd ** -0.5), k).astype(jnp.float32)
    p = jax.nn.softmax(sc, axis=-1).astype(v.dtype)
    return jnp.einsum('bhij,bhjd->bhid', p, v)


def axial_rope(t):
    n, hd = t.shape[2], t.shape[-1]
    half, nf = hd // 2, hd // 4
    pos = jnp.arange(n)
    p_row = (pos // GRID_W).astype(jnp.float32)
    p_col = (pos % GRID_W).astype(jnp.float32)
    inv = ROPE_BASE ** (-jnp.arange(nf, dtype=jnp.float32) / nf)

    def rot(a, p):
        ang = p[:, None] * inv[None, :]
        cos, sin = jnp.cos(ang), jnp.sin(ang)
        a1, a2 = a[..., :nf], a[..., nf:]
        return jnp.concatenate([a1 * cos - a2 * sin, a2 * cos + a1 * sin], axis=-1)

    t32 = t.astype(jnp.float32)
    out = jnp.concatenate([rot(t32[..., :half], p_row), rot(t32[..., half:], p_col)], axis=-1)
    return out.astype(t.dtype)


def retention_chunked(q, k, v, log_g, s0, include_diag):
    q, k, v = (t.astype(jnp.float32) for t in (q, k, v))
    b, h, n, dk = q.shape
    dv = v.shape[-1]
    nc = n // RET_CHUNK
    idx = jnp.arange(RET_CHUNK, dtype=jnp.float32)
    rel = idx[:, None] - idx[None, :]
    keep = rel >= 0 if include_diag else rel > 0
    dmat = jnp.where(keep, jnp.exp(log_g[:, None, None] * jnp.maximum(rel, 0.0)), 0.0)
    q_dec = jnp.exp(log_g[:, None] * (idx + 1.0))[None, :, :, None]
    k_dec = jnp.exp(log_g[:, None] * (RET_CHUNK - 1.0 - idx))[None, :, :, None]
    c_dec = jnp.exp(log_g * RET_CHUNK)[None, :, None, None]
    chunks = lambda t: t.reshape(b, h, nc, RET_CHUNK, t.shape[-1]).transpose(2, 0, 1, 3, 4)

    def step(s, qkv):
        qc, kc, vc = qkv
        inner = jnp.einsum('bhij,bhjv->bhiv', jnp.einsum('bhid,bhjd->bhij', qc, kc) * dmat, vc)
        cross = jnp.einsum('bhid,bhdv->bhiv', qc * q_dec, s)
        s = s * c_dec + jnp.einsum('bhjd,bhjv->bhdv', kc * k_dec, vc)
        return s, inner + cross

    _, o = lax.scan(step, s0.astype(jnp.float32), (chunks(q), chunks(k), chunks(v)))
    return o.transpose(1, 2, 0, 3, 4).reshape(b, h, n, dv)


def context_state(k, v, log_g, reverse):
    n = k.shape[2]
    pos = jnp.arange(n, dtype=jnp.float32)
    steps_after = pos if reverse else (n - 1.0) - pos
    w = jnp.exp(log_g[:, None] * steps_after)
    return jnp.einsum('bhld,bhlv->bhdv', k.astype(jnp.float32) * w[None, :, :, None], v.astype(jnp.float32))


def head_norm(o):
    mu = jnp.mean(o, axis=-1, keepdims=True)
    var = jnp.mean(jnp.square(o - mu), axis=-1, keepdims=True)
    return (o - mu) * lax.rsqrt(var + EPS)


def retention_branch(q, k, v, g, s_f, s_b, log_gf, log_gb):
    o_f = retention_chunked(q, k, v, log_gf, s_f, True)
    o_b = jnp.flip(retention_chunked(jnp.flip(q, 2), jnp.flip(k, 2), jnp.flip(v, 2), log_gb, s_b, False), 2)
    return from_heads(head_norm(o_f + o_b)).astype(g.dtype) * jax.nn.silu(g)


def merge_branches(parts, a, bb, r, w_a, w_b, w_c, w_o):
    y = (jax.nn.sigmoid(parts["gate_a"]) * (a @ w_a)
         + jax.nn.sigmoid(parts["gate_b"]) * (bb @ w_b)
         + jax.nn.sigmoid(parts["gate_c"]) * (r @ w_c))
    return y @ w_o


def token_mixer(h, hc, w_in, gm_norm_g, gm_ws, gm_bs, na_rpb, dec_f, dec_b, w_a, w_b, w_c, w_o, need_ctx):
    cols = _col_ranges()
    z_full = h @ w_in
    z = {name: z_full[..., lo:hi] for name, (lo, hi) in cols.items()}
    if need_ctx:
        zc_full = hc @ w_in
        zc = {name: zc_full[..., lo:hi] for name, (lo, hi) in cols.items()}
    else:
        zc = {name: hc @ w_in[:, cols[name][0]:cols[name][1]] for name in CTX_KV_NAMES}

    a = chunk_gmlp(z["gm_u"], z["gm_v"], gm_norm_g, gm_ws, gm_bs)

    nk_c, nv_c = to_heads(zc["na_k"], NA_HEADS), to_heads(zc["na_v"], NA_HEADS)
    bb = from_heads(neighbourhood_attention(to_heads(z["na_q"], NA_HEADS), to_heads(z["na_k"], NA_HEADS),
                                            to_heads(z["na_v"], NA_HEADS), nk_c, nv_c, na_rpb))

    k_scale = RET_DK ** -0.5
    log_gf = jnp.log1p(-jnp.exp2(dec_f.astype(jnp.float32)))
    log_gb = jnp.log1p(-jnp.exp2(dec_b.astype(jnp.float32)))
    rk_c = to_heads(zc["ret_k"], RET_HEADS) * k_scale
    rv_c = to_heads(zc["ret_v"], RET_HEADS)
    s_f = context_state(rk_c, rv_c, log_gf, reverse=False)
    s_b = context_state(rk_c, rv_c, log_gb, reverse=True)
    rq = axial_rope(to_heads(z["ret_q"], RET_HEADS))
    rk = axial_rope(to_heads(z["ret_k"], RET_HEADS)) * k_scale
    r = retention_branch(rq, rk, to_heads(z["ret_v"], RET_HEADS), z["ret_g"], s_f, s_b, log_gf, log_gb)

    out = merge_branches(z, a, bb, r, w_a, w_b, w_c, w_o)
    if not need_ctx:
        return out, None

    a_c = chunk_gmlp(zc["gm_u"], zc["gm_v"], gm_norm_g, gm_ws, gm_bs)
    b_c = from_heads(context_attention(to_heads(zc["na_q"], NA_HEADS), nk_c, nv_c))
    zeros = jnp.zeros_like(s_f)
    r_c = retention_branch(to_heads(zc["ret_q"], RET_HEADS), rk_c, rv_c, zc["ret_g"], zeros, zeros, log_gf, log_gb)
    out_c = merge_branches(zc, a_c, b_c, r_c, w_a, w_b, w_c, w_o)
    return out, out_c


def hierarchical_moe(hf, w_group, w_expert, w1, w3, w2):
    t = hf.shape[0]
    p_grp = jax.nn.softmax((hf @ w_group).astype(jnp.float32), axis=-1)
    grp = jnp.argmax(p_grp, axis=-1)
    p_sel = jnp.take_along_axis(p_grp, grp[:, None], axis=-1)
    le = (hf @ w_expert).astype(jnp.float32).reshape(t, MOE_GROUPS, MOE_EXPERTS_PER_GROUP)
    le = jnp.take_along_axis(le, grp[:, None, None], axis=1)[:, 0]
    top_w, top_i = lax.top_k(jax.nn.softmax(le, axis=-1), MOE_TOPK)
    weight = p_sel * top_w / jnp.sum(top_w, axis=-1, keepdims=True)
    eid = grp[:, None] * MOE_EXPERTS_PER_GROUP + top_i

    n_assign = t * MOE_TOPK
    flat_e = eid.reshape(-1).astype(jnp.int32)
    flat_t = jnp.repeat(jnp.arange(t, dtype=jnp.int32), MOE_TOPK)
    flat_w = weight.reshape(-1).astype(hf.dtype)
    order = jnp.argsort(flat_e)
    se, st, sw = flat_e[order], flat_t[order], flat_w[order]
    counts = jax.ops.segment_sum(jnp.ones_like(flat_e), flat_e, num_segments=MOE_EXPERTS)
    start = jnp.cumsum(counts) - counts
    padded = (counts + MOE_BLOCK - 1) // MOE_BLOCK * MOE_BLOCK
    p_end = jnp.cumsum(padded)
    p_start = p_end - padded
    dest = p_start[se] + jnp.arange(n_assign, dtype=jnp.int32) - start[se]
    n_blocks = (n_assign + MOE_EXPERTS * (MOE_BLOCK - 1)) // MOE_BLOCK
    n_rows = n_blocks * MOE_BLOCK
    row_tok = jnp.zeros((n_rows,), jnp.int32).at[dest].set(st)
    row_w = jnp.zeros((n_rows,), hf.dtype).at[dest].set(sw)
    block_e = jnp.clip(jnp.searchsorted(p_end, jnp.arange(n_blocks) * MOE_BLOCK, side='right'), 0, MOE_EXPERTS - 1)

    def run_block(blk):
        e, toks, wts = blk
        xb = hf[toks]
        y = (jax.nn.silu(xb @ w1[e]) * (xb @ w3[e])) @ w2[e]
        return y * wts[:, None]

    ys = lax.map(run_block, (block_e, row_tok.reshape(n_blocks, MOE_BLOCK), row_w.reshape(n_blocks, MOE_BLOCK)))
    return jnp.zeros_like(hf).at[row_tok].add(ys.reshape(n_rows, -1))


def setup_inputs(seed: int = 0) -> dict:
    key = jax.random.key(seed)
    ks = jax.random.split(key, 32)
    f32 = jnp.float32
    nrm = lambda k, shape, scale: jax.random.normal(k, shape, f32) * scale
    d = D_MODEL
    decay_base = -(RET_DECAY_BASE + jnp.arange(RET_HEADS, dtype=f32))
    return {
        "x": nrm(ks[0], (BATCH, SEQ, d), 1.0),
        "c": nrm(ks[1], (BATCH, d), 1.0),
        "ctx": nrm(ks[2], (BATCH, CTX_LEN, d), 1.0),
        "c_ctx": nrm(ks[3], (d,), 1.0),
        "ada_w": nrm(ks[4], (DEPTH, d, 6 * d), 0.5 * d ** -0.5),
        "ada_b": nrm(ks[5], (DEPTH, 6 * d), 0.02),
        "norm1_g": 1.0 + nrm(ks[6], (DEPTH, d), 0.02),
        "w_in": nrm(ks[7], (DEPTH, d, IN_TOTAL), d ** -0.5),
        "gm_norm_g": 1.0 + nrm(ks[8], (DEPTH, GM_WIDTH), 0.02),
        "gm_ws": nrm(ks[9], (DEPTH, GM_GROUPS, GM_CHUNK, GM_CHUNK), GM_CHUNK ** -0.5),
        "gm_bs": 1.0 + nrm(ks[10], (DEPTH, GM_GROUPS, GM_CHUNK), 0.02),
        "na_rpb": nrm(ks[11], (DEPTH, NA_HEADS, 2 * NA_ROWS - 1, 2 * NA_COLS - 1), 0.1),
        "ret_decay_fwd": decay_base + nrm(ks[12], (DEPTH, RET_HEADS), 0.1),
        "ret_decay_bwd": decay_base + nrm(ks[13], (DEPTH, RET_HEADS), 0.1),
        "w_branch_a": nrm(ks[14], (DEPTH, GM_WIDTH, d), GM_WIDTH ** -0.5),
        "w_branch_b": nrm(ks[15], (DEPTH, NA_WIDTH, d), NA_WIDTH ** -0.5),
        "w_branch_c": nrm(ks[16], (DEPTH, RET_V_WIDTH, d), RET_V_WIDTH ** -0.5),
        "w_out": nrm(ks[17], (DEPTH, d, d), d ** -0.5),
        "norm2_g": 1.0 + nrm(ks[18], (DEPTH, d), 0.02),
        "moe_w_group": nrm(ks[19], (DEPTH, d, MOE_GROUPS), d ** -0.5),
        "moe_w_expert": nrm(ks[20], (DEPTH, d, MOE_EXPERTS), d ** -0.5),
        "moe_w1": nrm(ks[21], (DEPTH, MOE_EXPERTS, d, MOE_HIDDEN), d ** -0.5),
        "moe_w3": nrm(ks[22], (DEPTH, MOE_EXPERTS, d, MOE_HIDDEN), d ** -0.5),
        "moe_w2": nrm(ks[23], (DEPTH, MOE_EXPERTS, MOE_HIDDEN, d), MOE_HIDDEN ** -0.5),
        "final_norm_g": 1.0 + nrm(ks[24], (d,), 0.02),
    }


def reference(x, c, ctx, c_ctx, ada_w, ada_b, norm1_g, w_in, gm_norm_g, gm_ws, gm_bs, na_rpb,
              ret_decay_fwd, ret_decay_bwd, w_branch_a, w_branch_b, w_branch_c, w_out, norm2_g,
              moe_w_group, moe_w_expert, moe_w1, moe_w3, moe_w2, final_norm_g):
    b, s, d = x.shape
    n_ctx = ctx.shape[1]
    xc = ctx
    for l in range(DEPTH):
        need_ctx = l < DEPTH - 1
        mod = jax.nn.silu(c) @ ada_w[l] + ada_b[l]
        mod_c = jax.nn.silu(c_ctx) @ ada_w[l] + ada_b[l]
        sh1, sc1, g1, sh2, sc2, g2 = jnp.split(mod[:, None, :], 6, axis=-1)
        sh1c, sc1c, g1c, sh2c, sc2c, g2c = jnp.split(mod_c, 6)

        h = modulate(rms_norm(x, norm1_g[l]), sh1, sc1)
        hc = modulate(rms_norm(xc, norm1_g[l]), sh1c, sc1c)
        out, out_c = token_mixer(h, hc, w_in[l], gm_norm_g[l], gm_ws[l], gm_bs[l], na_rpb[l],
                                 ret_decay_fwd[l], ret_decay_bwd[l], w_branch_a[l], w_branch_b[l],
                                 w_branch_c[l], w_out[l], need_ctx)
        x = x + g1 * out
        h2 = modulate(rms_norm(x, norm2_g[l]), sh2, sc2).reshape(b * s, d)
        if need_ctx:
            xc = xc + g1c * out_c
            h2c = modulate(rms_norm(xc, norm2_g[l]), sh2c, sc2c).reshape(b * n_ctx, d)
            f = hierarchical_moe(jnp.concatenate([h2, h2c], axis=0), moe_w_group[l], moe_w_expert[l],
                                 moe_w1[l], moe_w3[l], moe_w2[l])
            x = x + g2 * f[:b * s].reshape(b, s, d)
            xc = xc + g2c * f[b * s:].reshape(b, n_ctx, d)
        else:
            f = hierarchical_moe(h2, moe_w_group[l], moe_w_expert[l], moe_w1[l], moe_w3[l], moe_w2[l])
            x = x + g2 * f.reshape(b, s, d)
    return rms_norm(x, final_norm_g)
```

```python
import math
import numpy as np
from contextlib import ExitStack
import concourse.bass as bass
import concourse.mybir as mybir
from concourse.bass_utils import run_bass_kernel_spmd

F32 = mybir.dt.float32
BF16 = mybir.dt.bfloat16
I32 = mybir.dt.int32
AF = mybir.ActivationFunctionType
ALU = mybir.AluOpType
AX = mybir.AxisListType

NEG = -1e30
EPS = 1e-6


class Buf:
    __slots__ = ("name", "w", "r")

    def __init__(self, name):
        self.name = name
        self.w = None
        self.r = []


class Prog:
    ENGS = ("pe", "act", "dve", "pool", "sp")

    def __init__(self, nc, stack, sbuf_bytes=192 * 1024, self_sync=True):
        self.nc = nc
        self.stack = stack
        self.ops = {e: [] for e in self.ENGS}
        self.cnt = {e: 0 for e in self.ENGS}
        self.waited = {e: {} for e in self.ENGS}
        self.sem = {}
        self.semval = {}
        self.self_sync = self_sync
        for e in self.ENGS:
            self._mksem("E_" + e)
        self.arena_words = sbuf_bytes // 4
        self.arena = stack.enter_context(nc.sbuf_tensor("arena", [128, self.arena_words], F32))
        self.top = 0
        self.marks = []
        self.psum = [stack.enter_context(nc.psum_tensor(f"ps{i}", [128, 512], F32)) for i in range(8)]
        self.psb = [Buf(f"ps{i}") for i in range(8)]
        self.groups = {}
        self.dcount = {}
        self.dbufs = {}

    def _mksem(self, key):
        self.sem[key] = self.stack.enter_context(self.nc.semaphore(key))
        self.semval[key] = 0

    def mark(self):
        self.marks.append(self.top)

    def release(self):
        self.top = self.marks.pop()

    def tile(self, shape, dtype, name="t"):
        free = int(np.prod(shape[1:]))
        esz = 2 if dtype == BF16 else 4
        words = (free * esz + 3) // 4
        words = (words + 7) // 8 * 8
        off = self.top
        self.top += words
        assert self.top <= self.arena_words, f"SBUF arena overflow {self.top * 4} ({name})"
        ap = self.arena[:, off:off + words]
        if dtype != F32:
            ap = ap.bitcast(dtype)
        ap = ap[:, 0:free]
        if len(shape) > 2:
            names = " ".join(f"d{i}" for i in range(1, len(shape)))
            kw = {f"d{i}": shape[i] for i in range(1, len(shape))}
            ap = ap.rearrange(f"p ({names}) -> p {names}", **kw)
        if shape[0] < 128:
            ap = ap[0:shape[0]]
        return ap, Buf(name)

    def ps(self, i, dtype=F32):
        t = self.psum[i][:, :]
        if dtype != F32:
            t = t.bitcast(dtype)
        return t

    def dbuf(self, *key):
        if key not in self.dbufs:
            self.dbufs[key] = Buf(str(key))
        return self.dbufs[key]

    def _deps(self, eng, reads, writes):
        deps = {}

        def add(ev):
            if ev is None:
                return
            k, v = ev
            if deps.get(k, 0) < v:
                deps[k] = v
        for b in reads:
            add(b.w)
        for b in writes:
            add(b.w)
            for ev in b.r:
                add(ev)
        waits = []
        own = "E_" + eng
        for k, v in deps.items():
            if k == own and (eng in ("pe", "sp") or not self.self_sync):
                continue
            if self.waited[eng].get(k, 0) < v:
                self.waited[eng][k] = v
                waits.append((k, v))
        return waits

    def _commit(self, ev, reads, writes):
        for b in reads:
            if len(b.r) > 24:
                m = {}
                for k, v in b.r:
                    if m.get(k, 0) < v:
                        m[k] = v
                b.r = list(m.items())
            b.r.append(ev)
        for b in writes:
            b.w = ev
            b.r = []

    def op(self, eng, fn, reads=(), writes=()):
        waits = self._deps(eng, reads, writes)
        self.cnt[eng] += 1
        key = "E_" + eng
        ev = (key, self.cnt[eng])
        self.ops[eng].append((waits, fn, key, 1))
        self._commit(ev, reads, writes)
        return ev

    NQ = {"sp": 56, "pool": 28, "act": 2}

    def dma(self, q, fn, reads=(), writes=(), grp="g0"):
        waits = self._deps(q, reads, writes)
        n = self.dcount.get(q, 0)
        self.dcount[q] = n + 1
        key = f"Q_{q}_{n % self.NQ[q]}"
        if key not in self.sem:
            self._mksem(key)
            self.groups[key] = key
        prev = self.semval[key]
        if prev > 0 and self.waited[q].get(key, 0) < prev:
            self.waited[q][key] = prev
            waits.append((key, prev))
        self.semval[key] += 16
        ev = (key, self.semval[key])
        self.ops[q].append((waits, fn, key, 16))
        self._commit(ev, reads, writes)
        return ev

    def barrier(self):
        evs = [("E_" + e, self.cnt[e]) for e in self.ENGS if self.cnt[e] > 0]
        evs += [(k, self.semval[k]) for k in self.groups.values() if self.semval[k] > 0]
        for e in self.ENGS:
            waits = []
            for k, v in evs:
                if k == "E_" + e:
                    continue
                if self.waited[e].get(k, 0) < v:
                    self.waited[e][k] = v
                    waits.append((k, v))
            if waits:
                self.ops[e].append((waits, None, None, 0))

    def emit(self):
        nc = self.nc
        self.barrier()
        prog = self

        class Rec:
            def __init__(self, eng):
                self._eng = eng
                self.first = None

            def __getattr__(self, name):
                f = getattr(self._eng, name)

                def g(*a, **kw):
                    r = f(*a, **kw)
                    if self.first is None:
                        self.first = r
                    return r
                return g

        def run(engobj, lst):
            for waits, fn, key, inc in lst:
                if fn is None:
                    for k, v in waits:
                        engobj.wait_ge(prog.sem[k], v)
                    continue
                for k, v in waits[1:]:
                    engobj.wait_ge(prog.sem[k], v)
                rec = Rec(engobj)
                ins = fn(rec)
                if waits:
                    k, v = waits[0]
                    rec.first._wait_ge(prog.sem[k], v)
                ins.then_inc(prog.sem[key], inc)

        with nc.Block() as block:
            @block.tensor
            def _(t):
                run(t, prog.ops["pe"])

            @block.scalar
            def _(s):
                run(s, prog.ops["act"])

            @block.vector
            def _(v):
                run(v, prog.ops["dve"])

            @block.gpsimd
            def _(g):
                run(g, prog.ops["pool"])

            @block.sync
            def _(s):
                run(s, prog.ops["sp"])


def V(name, *a, **kw):
    return lambda e: getattr(e, name)(*a, **kw)


class Cfg:
    def __init__(self, D=4096, S=8192, L=256, depth=2, MH=512, CAP=1920):
        self.D, self.S, self.L, self.depth, self.MH, self.CAP = D, S, L, depth, MH, CAP
        self.HD = 128
        self.NH = 8
        self.GW = 64
        self.T = S + L
        self.NTL = S // 128
        self.NTC = L // 128
        self.NT = self.NTL + self.NTC
        self.KD = D // 128
        self.ROWS = S // 64
        self.NE = 32
        D_ = D
        names = [("gate_a", D_), ("gate_b", D_), ("gate_c", D_), ("gm_u", 1024), ("gm_v", 1024),
                 ("na_q", 1024), ("na_k", 1024), ("na_v", 1024), ("ret_q", 1024), ("ret_k", 1024),
                 ("ret_v", 2048), ("ret_g", 2048)]
        self.cols = {}
        off = 0
        for n, w in names:
            self.cols[n] = (off, off + w)
            off += w
        self.IN = off
        self.ZM0 = 3 * D_
        self.ZMW = off - 3 * D_
        self.NSLOT = self.NE * CAP
        self.TRASH = self.NSLOT
        self.HALF = (self.NE // 2) * CAP


def na_variants(cfg):
    rows = cfg.ROWS
    kr = min(8, rows)
    WT = 5 if rows >= 10 else rows // 2
    variants = {}
    tile_var = []
    tile_a = []
    for qt in range(rows // 2):
        r = 2 * qt
        a = int(np.clip(r - 4, 0, rows - 2 * WT))
        a -= a % 2
        key = []
        for rr in (r, r + 1):
            rs = int(np.clip(rr - kr // 2, 0, rows - kr))
            key.append((rs - a, rr - a))
        key = tuple(key)
        if key not in variants:
            variants[key] = len(variants)
        tile_var.append(variants[key])
        tile_a.append(a)
    nv = len(variants)
    nk = WT * 128
    dr_idx = np.zeros((nv, 128, nk), np.int64)
    dc_idx = np.zeros((nv, 128, nk), np.int64)
    mask = np.full((nv, 128, nk), NEG, np.float32)
    for key, vi in variants.items():
        for half, (rs_rel, rr_rel) in enumerate(key):
            for c in range(64):
                p = half * 64 + c
                c_start = int(np.clip(c - 8, 0, 64 - 16))
                for jj in range(2 * WT):
                    for w in range(64):
                        kidx = jj * 64 + w
                        dr = jj - rr_rel + 7
                        dc = int(np.clip(w - c + 15, 0, 30))
                        ok = (rs_rel <= jj < rs_rel + kr) and (c_start <= w < c_start + 16)
                        if ok:
                            dr_idx[vi, p, kidx] = dr
                            dc_idx[vi, p, kidx] = dc
                            mask[vi, p, kidx] = 0.0
    return WT, nv, tile_var, tile_a, dr_idx, dc_idx, mask


def build(cfg, stop_after=None, dbg=None):
    D, S, L, T, KD, NT, NTL, NTC = cfg.D, cfg.S, cfg.L, cfg.T, cfg.KD, cfg.NT, cfg.NTL, cfg.NTC
    depth, MH, CAP, NE = cfg.depth, cfg.MH, cfg.CAP, cfg.NE
    KH = MH // 128
    NBD = D // 512
    WT, NV, tile_var, tile_a, _, _, _ = na_variants(cfg)
    NKL = WT * 128

    nc = bass.Bass("TRN2", target_bir_lowering=False)

    def din(name, shape, dt=F32):
        return nc.dram_tensor(name, list(shape), dt, kind="ExternalInput").ap()

    def dscr(name, shape, dt):
        return nc.dram_tensor(name, list(shape), dt).ap()

    x_in = din("x", [S, D])
    ctx_in = din("ctx", [L, D])
    cvec = din("cvec", [2, D])
    ada_w = din("ada_w", [depth, D, 6 * D])
    ada_b = din("ada_b", [depth, 6 * D])
    norm1_g = din("norm1_g", [depth, D])
    w_in = din("w_in", [depth, D, cfg.IN])
    gm_norm_g = din("gm_norm_g", [depth, 1024])
    gm_ws = din("gm_ws", [depth, 8, 128, 128])
    gm_bsT = din("gm_bsT", [depth, 128, 8])
    na_bias = din("na_bias", [depth, 8, NV, 128, NKL])
    dec_f = din("ret_decay_fwd", [depth, 8])
    dec_b = din("ret_decay_bwd", [depth, 8])
    w_a = din("w_branch_a", [depth, 1024, D])
    w_b = din("w_branch_b", [depth, 1024, D])
    w_c = din("w_branch_c", [depth, 2048, D])
    w_out = din("w_out", [depth, D, D])
    norm2_g = din("norm2_g", [depth, D])
    w_router = din("w_router", [depth, D, 36])
    moe_w1 = din("moe_w1", [depth, NE, D, MH])
    moe_w3 = din("moe_w3", [depth, NE, D, MH])
    moe_w2 = din("moe_w2", [depth, NE, MH, D])
    final_g = din("final_norm_g", [D])
    c_ident = din("c_ident", [128, 128])
    c_rope = din("c_rope", [NTL, 128, 128])
    c_rel = din("c_rel", [128, 4, 128])
    c_idx = din("c_idx", [128, 4, 128])
    c_tri = din("c_tri", [128, 2, 128])
    c_ecap = din("c_ecap", [128, 32])
    c_tok = din("c_tok", [NT, 128, 4], I32)
    c_tabinit = din("c_tabinit", [128, (cfg.NSLOT + 128) // 128 * 4], I32)
    c_zero = din("c_zero", [128, D], BF16)

    out_d = nc.dram_tensor("out", [S, D], F32, kind="ExternalOutput").ap()
    dbg_d = None
    if dbg is not None:
        dbg_d = nc.dram_tensor("dbg", list(dbg[1]), dbg[2], kind="ExternalOutput").ap()

    xres = dscr("xres", [T, D], F32)
    modd = dscr("modd", [depth, 2, 6 * D], F32)
    hT_d = dscr("hT_d", [NT, 128, KD, 128], BF16)
    yT_d = dscr("yT_d", [NT, 128, KD, 128], BF16)
    abrT_d = dscr("abrT_d", [NT, 128, 32, 128], BF16)
    zg_d = dscr("zg_d", [T, 3 * D], BF16)
    zm_d = dscr("zm_d", [T, cfg.ZMW], BF16)
    sb_d = dscr("sb_d", [NT, 128, 8, 256], BF16)
    h2_d = dscr("h2_d", [T + 128, D], BF16)
    ys_a = dscr("ys_a", [cfg.HALF + 128, D], BF16)
    ys_b = dscr("ys_b", [cfg.HALF + 128, D], BF16)
    tab_d = dscr("tab_d", [cfg.NSLOT + 128, 4], I32)

    st = ExitStack()
    P = Prog(nc, st)
    nc_allow = nc.allow_non_contiguous_dma(reason="small strided loads")
    st.enter_context(nc_allow)

    def zc(name):
        lo, hi = cfg.cols[name]
        return lo - cfg.ZM0, hi - cfg.ZM0

    identf, identf_b = P.tile([128, 128], F32, "identf")
    identb, identb_b = P.tile([128, 128], BF16, "identb")
    P.dma("sp", V("dma_start", out=identf, in_=c_ident), writes=[identf_b], grp="const")
    P.op("dve", V("tensor_copy", out=identb, in_=identf), reads=[identf_b], writes=[identb_b])
    epsb, epsb_b = P.tile([128, 1], F32, "epsb")
    P.op("dve", V("memset", epsb, EPS), writes=[epsb_b])

    def transpose_to(src, src_b, dst, dst_b, nchunk, banks, evac=("act", "dve"), dt=BF16, ident=None, ident_b=None):
        if ident is None:
            ident, ident_b = (identb, identb_b) if dt == BF16 else (identf, identf_b)
        per = 8 if dt == BF16 else 4
        c = 0
        i = 0
        while c < nchunk:
            n = min(per, nchunk - c)
            bk = banks[i % len(banks)]
            pst = P.ps(bk, dt)

            def tr(e, c=c, n=n, pst=pst):
                ins = None
                for k in range(n):
                    ins = e.transpose(out=pst[:, k * 128:(k + 1) * 128], in_=src[:, (c + k) * 128:(c + k + 1) * 128],
                                      identity=ident)
                return ins
            P.op("pe", tr, reads=[src_b, ident_b], writes=[P.psb[bk]])
            eng = evac[i % len(evac)]
            dview = dst[:, c:c + n, :].rearrange("p a b -> p (a b)")
            if eng == "act":
                P.op("act", V("copy", out=dview, in_=pst[:, 0:n * 128]), reads=[P.psb[bk]], writes=[dst_b])
            else:
                P.op("dve", V("tensor_copy", out=dview, in_=pst[:, 0:n * 128]), reads=[P.psb[bk]], writes=[dst_b])
            c += n
            i += 1

    def bcast_row(dram_row_ap, n):
        return dram_row_ap.partition_broadcast(128)

    P.dma("sp", V("dma_start", out=xres[0:S, :], in_=x_in), writes=[P.dbuf("xres", "all")], grp="init")
    P.dma("sp", V("dma_start", out=xres[S:T, :], in_=ctx_in), writes=[P.dbuf("xres", "all")], grp="init")
    P.dma("sp", V("dma_start", out=h2_d[T:T + 128, :], in_=c_zero), writes=[P.dbuf("h2pad")], grp="init")
    P.dma("sp", V("dma_start", out=ys_a[cfg.HALF:cfg.HALF + 128, :], in_=c_zero), writes=[P.dbuf("ysapad")], grp="init")
    P.dma("sp", V("dma_start", out=ys_b[cfg.HALF:cfg.HALF + 128, :], in_=c_zero), writes=[P.dbuf("ysbpad")], grp="init")

    def phase_mod():
        P.mark()
        cv, cv_b = P.tile([2, D], F32, "cv")
        P.dma("sp", V("dma_start", out=cv, in_=cvec), writes=[cv_b], grp="ld")
        sc, sc_b = P.tile([2, D], F32, "sc")
        P.op("act", V("activation", out=sc, in_=cv, func=AF.Silu), reads=[cv_b], writes=[sc_b])
        scT, scT_b = P.tile([128, KD, 2], BF16, "scT")
        for c0 in range(0, KD, 64):
            n = min(64, KD - c0)
            pst = P.ps(0)

            def tr(e, c0=c0, n=n, pst=pst):
                ins = None
                for k in range(n):
                    ins = e.transpose(out=pst[:, k * 2:(k + 1) * 2], in_=sc[:, (c0 + k) * 128:(c0 + k + 1) * 128],
                                      identity=identf[0:2, 0:2])
                return ins
            P.op("pe", tr, reads=[sc_b, identf_b], writes=[P.psb[0]])
            P.op("dve", V("tensor_copy", out=scT[:, c0:c0 + n, :].rearrange("p a b -> p (a b)"), in_=pst[:, 0:2 * n]),
                 reads=[P.psb[0]], writes=[scT_b])
        wb = [P.tile([128, KD, 512], BF16, f"adaw{i}") for i in range(2)]
        bb = [P.tile([2, 512], F32, f"adab{i}") for i in range(2)]
        ob = [P.tile([2, 512], F32, f"modo{i}") for i in range(2)]
        it = 0
        for l in range(depth):
            for nb in range(6 * D // 512):
                w, w_bf = wb[it % 2]
                bt, bt_b = bb[it % 2]
                ot, ot_b = ob[it % 2]
                P.dma("pool", V("dma_start", out=w, in_=ada_w[l, :, nb * 512:(nb + 1) * 512].rearrange("(k p) n -> p k n", p=128)),
                      writes=[w_bf], grp="w")
                P.dma("sp", V("dma_start", out=bt, in_=ada_b[l, nb * 512:(nb + 1) * 512].partition_broadcast(2)),
                      writes=[bt_b], grp="ld")
                bk = 1 + it % 2
                pso = P.ps(bk)

                def mm(e, w=w, pso=pso):
                    ins = None
                    for k in range(KD):
                        ins = e.matmul(pso[0:2, :], lhsT=scT[:, k, :], rhs=w[:, k, :], start=(k == 0), stop=(k == KD - 1))
                    return ins
                P.op("pe", mm, reads=[scT_b, w_bf], writes=[P.psb[bk]])
                P.op("dve", V("tensor_tensor", out=ot, in0=pso[0:2, :], in1=bt, op=ALU.add),
                     reads=[P.psb[bk], bt_b], writes=[ot_b])
                P.dma("sp", V("dma_start", out=modd[l, :, nb * 512:(nb + 1) * 512], in_=ot), reads=[ot_b],
                      writes=[P.dbuf("modd")], grp="st")
                it += 1
        P.release()
        P.barrier()

    phase_mod()

    def modrow(l, r, i):
        return modd[l, r, i * D:(i + 1) * D]

    def phase_norm(l, which, tiles, route=None):
        P.mark()
        gsrc = norm1_g if which == 0 else norm2_g
        sh_i, sc_i = (0, 1) if which == 0 else (3, 4)
        geff = []
        shf = []
        for r in range(2):
            g_t, g_b = P.tile([128, D], F32, f"geff{r}")
            s_t, s_b = P.tile([128, D], F32, f"shf{r}")
            P.dma("sp", V("dma_start", out=g_t, in_=modrow(l, r, sc_i).partition_broadcast(128)), reads=[P.dbuf("modd")],
                  writes=[g_b], grp="ld")
            P.dma("sp", V("dma_start", out=s_t, in_=gsrc[l].partition_broadcast(128)), writes=[s_b], grp="ld")
            P.op("dve", V("scalar_tensor_tensor", out=g_t, in0=g_t, scalar=1.0, in1=s_t, op0=ALU.add, op1=ALU.mult),
                 reads=[g_b, s_b], writes=[g_b])
            P.dma("sp", V("dma_start", out=s_t, in_=modrow(l, r, sh_i).partition_broadcast(128)), reads=[P.dbuf("modd")],
                  writes=[s_b], grp="ld")
            geff.append((g_t, g_b))
            shf.append((s_t, s_b))
        xb = [P.tile([128, D], F32, f"xn{i}") for i in range(2)]
        junk, junk_b = P.tile([128, D], BF16, "junk")
        st_ = [P.tile([128, 4], F32, f"nst{i}") for i in range(2)]
        if which == 0:
            hb = [P.tile([128, D], BF16, f"hb{i}") for i in range(2)]
            hT = [P.tile([128, KD, 128], BF16, f"hT{i}") for i in range(2)]
        else:
            hf = [P.tile([128, D], F32, f"hf{i}") for i in range(2)]
            hb = [P.tile([128, D], BF16, f"hb{i}") for i in range(2)]
            hTf, hTf_b = P.tile([128, KD, 128], F32, "hTf")
            wr, wr_b = P.tile([128, KD, 36], F32, "wr")
            P.dma("sp", V("dma_start", out=wr, in_=w_router[l].rearrange("(k p) n -> p k n", p=128)), writes=[wr_b], grp="ld")
            tri, tri_b = P.tile([128, 2, 128], F32, "tri")
            P.dma("sp", V("dma_start", out=tri, in_=c_tri), writes=[tri_b], grp="ld")
            ecap, ecap_b = P.tile([128, 32], F32, "ecap")
            P.dma("sp", V("dma_start", out=ecap, in_=c_ecap), writes=[ecap_b], grp="ld")
            base, base_b = P.tile([128, 32], F32, "base")
            P.op("dve", V("memset", base, 0.0), writes=[base_b])
            rt = [dict((k, P.tile(s, d, f"rt_{k}{i}")) for k, s, d in (
                ("lg", [128, 36], F32), ("sm", [128, 16], F32), ("ohg", [128, 4], F32), ("le", [128, 8], F32),
                ("m8", [128, 8], F32), ("oh", [128, 2, 8], F32), ("A", [128, 3, 32], F32), ("pos", [128, 32], F32),
                ("tmp", [128, 32], F32), ("di", [128, 2, 4], I32), ("tok", [128, 4], I32), ("eg", [128, 4], F32))) for i in range(2)]
            tinit, tinit_b = P.tile([128, (cfg.NSLOT + 128) // 128 * 4], I32, "tinit")
            P.dma("sp", V("dma_start", out=tinit, in_=c_tabinit), writes=[tinit_b], grp="ld")
            P.dma("sp", V("dma_start", out=tab_d.rearrange("(p n) c -> p (n c)", p=128), in_=tinit), reads=[tinit_b],
                  writes=[P.dbuf("tab")], grp="st")
        for i, t in enumerate(tiles):
            r = 0 if t < NTL else 1
            g_t, g_b = geff[r]
            s_t, s_b = shf[r]
            x_t, x_b = xb[i % 2]
            sq, sq_b = st_[i % 2]
            P.dma("sp", V("dma_start", out=x_t, in_=xres[t * 128:(t + 1) * 128, :]),
                  reads=[P.dbuf("xres", "all"), P.dbuf("xres", t)], writes=[x_b], grp="ld")
            P.op("act", V("activation", out=junk, in_=x_t, func=AF.Square, accum_out=sq[:, 0:1]), reads=[x_b],
                 writes=[junk_b, sq_b])
            P.op("act", V("activation", out=sq[:, 1:2], in_=sq[:, 0:1], func=AF.Sqrt, scale=1.0 / D, bias=epsb[:, 0:1]),
                 reads=[sq_b, epsb_b], writes=[sq_b])
            P.op("dve", V("reciprocal", out=sq[:, 2:3], in_=sq[:, 1:2]), reads=[sq_b], writes=[sq_b])
            P.op("dve", V("scalar_tensor_tensor", out=x_t, in0=x_t, scalar=sq[:, 2:3], in1=g_t, op0=ALU.mult, op1=ALU.mult),
                 reads=[x_b, sq_b, g_b], writes=[x_b])
            h_t, h_b = hb[i % 2]
            if which == 0:
                P.op("pool", V("tensor_tensor", out=h_t, in0=x_t, in1=s_t, op=ALU.add), reads=[x_b, s_b], writes=[h_b])
                hT_t, hT_b = hT[i % 2]
                transpose_to(h_t, h_b, hT_t, hT_b, KD, banks=[0, 1, 2, 3])
                P.dma("sp", V("dma_start", out=hT_d[t], in_=hT_t), reads=[hT_b], writes=[P.dbuf("hT", t)], grp="st")
            else:
                f_t, f_b = hf[i % 2]
                P.op("pool", V("tensor_tensor", out=f_t, in0=x_t, in1=s_t, op=ALU.add), reads=[x_b, s_b], writes=[f_b])
                P.op("act", V("copy", out=h_t, in_=f_t), reads=[f_b], writes=[h_b])
                P.dma("sp", V("dma_start", out=h2_d[t * 128:(t + 1) * 128, :], in_=h_t), reads=[h_b],
                      writes=[P.dbuf("h2", t)], grp="st")
                transpose_to(f_t, f_b, hTf, hTf_b, KD, banks=[0, 1, 2, 3], dt=F32)
                R = rt[i % 2]
                pl = P.ps(4)

                def mmr(e, pl=pl):
                    ins = None
                    for k in range(KD):
                        ins = e.matmul(pl[:, 0:36], lhsT=hTf[:, k, :], rhs=wr[:, k, :], start=(k == 0), stop=(k == KD - 1))
                    return ins
                P.op("pe", mmr, reads=[hTf_b, wr_b], writes=[P.psb[4]])
                lg, lg_b = R["lg"]
                sm, sm_b = R["sm"]
                ohg, ohg_b = R["ohg"]
                le, le_b = R["le"]
                m8, m8_b = R["m8"]
                oh, oh_b = R["oh"]
                A, A_b = R["A"]
                pos, pos_b = R["pos"]
                tmp, tmp_b = R["tmp"]
                di, di_b = R["di"]
                tok, tok_b = R["tok"]
                eg, eg_b = R["eg"]
                P.op("dve", V("tensor_copy", out=lg, in_=pl[:, 0:36]), reads=[P.psb[4]], writes=[lg_b])
                P.op("dve", V("reduce_max", out=sm[:, 0:1], in_=lg[:, 0:4], axis=AX.X), reads=[lg_b], writes=[sm_b])
                P.op("dve", V("tensor_scalar", out=ohg, in0=lg[:, 0:4], scalar1=sm[:, 0:1], scalar2=None, op0=ALU.is_equal),
                     reads=[lg_b, sm_b], writes=[ohg_b])
                P.op("dve", V("tensor_scalar", out=sm[:, 1:2], in0=sm[:, 0:1], scalar1=-1.0, scalar2=None, op0=ALU.mult),
                     reads=[sm_b], writes=[sm_b])
                P.op("act", V("activation", out=eg, in_=lg[:, 0:4], func=AF.Exp, bias=sm[:, 1:2], accum_out=sm[:, 2:3]),
                     reads=[lg_b, sm_b], writes=[eg_b, sm_b])
                P.op("dve", V("reciprocal", out=sm[:, 3:4], in_=sm[:, 2:3]), reads=[sm_b], writes=[sm_b])
                P.op("dve", V("tensor_scalar", out=le, in0=lg[:, 4:12], scalar1=ohg[:, 0:1], scalar2=None, op0=ALU.mult),
                     reads=[lg_b, ohg_b], writes=[le_b])
                for g in range(1, 4):
                    P.op("dve", V("scalar_tensor_tensor", out=le, in0=lg[:, 4 + 8 * g:12 + 8 * g], scalar=ohg[:, g:g + 1], in1=le,
                                  op0=ALU.mult, op1=ALU.add), reads=[lg_b, ohg_b, le_b], writes=[le_b])
                P.op("dve", V("max", out=m8, in_=le), reads=[le_b], writes=[m8_b])
                for k in range(2):
                    P.op("dve", V("tensor_scalar", out=oh[:, k, :], in0=le, scalar1=m8[:, k:k + 1], scalar2=None, op0=ALU.is_equal),
                         reads=[le_b, m8_b], writes=[oh_b])
                P.op("dve", V("tensor_tensor", out=sm[:, 4:5], in0=m8[:, 1:2], in1=m8[:, 0:1], op=ALU.subtract),
                     reads=[m8_b, sm_b], writes=[sm_b])
                P.op("act", V("activation", out=sm[:, 5:6], in_=sm[:, 4:5], func=AF.Exp), reads=[sm_b], writes=[sm_b])
                P.op("dve", V("tensor_scalar", out=sm[:, 6:7], in0=sm[:, 5:6], scalar1=1.0, scalar2=None, op0=ALU.add),
                     reads=[sm_b], writes=[sm_b])
                P.op("dve", V("reciprocal", out=sm[:, 7:8], in_=sm[:, 6:7]), reads=[sm_b], writes=[sm_b])
                rw, rw_b = route["w"]
                P.op("dve", V("tensor_tensor", out=rw[:, t, 0:1], in0=sm[:, 7:8], in1=sm[:, 3:4], op=ALU.mult),
                     reads=[sm_b], writes=[rw_b])
                P.op("dve", V("tensor_tensor", out=rw[:, t, 1:2], in0=sm[:, 3:4], in1=rw[:, t, 0:1], op=ALU.subtract),
                     reads=[sm_b, rw_b], writes=[rw_b])
                for k in range(2):
                    for g in range(4):
                        P.op("dve", V("tensor_scalar", out=A[:, k, 8 * g:8 * g + 8], in0=oh[:, k, :], scalar1=ohg[:, g:g + 1],
                                      scalar2=None, op0=ALU.mult), reads=[oh_b, ohg_b], writes=[A_b])
                P.op("dve", V("tensor_tensor", out=A[:, 2, :], in0=A[:, 0, :], in1=A[:, 1, :], op=ALU.add), reads=[A_b], writes=[A_b])
                pp = P.ps(5)

                def mmp(e, pp=pp, A=A):
                    e.matmul(pp[:, 0:32], lhsT=tri[:, 0, :], rhs=A[:, 2, :], start=True, stop=True)
                    return e.matmul(pp[:, 32:64], lhsT=tri[:, 1, :], rhs=A[:, 2, :], start=True, stop=True)
                P.op("pe", mmp, reads=[tri_b, A_b], writes=[P.psb[5]])
                P.op("dve", V("tensor_tensor", out=pos, in0=pp[:, 0:32], in1=base, op=ALU.add), reads=[P.psb[5], base_b],
                     writes=[pos_b])
                P.op("dve", V("tensor_tensor", out=base, in0=pp[:, 32:64], in1=base, op=ALU.add), reads=[P.psb[5], base_b],
                     writes=[base_b])
                for k in range(2):
                    P.op("dve", V("tensor_tensor", out=tmp, in0=A[:, k, :], in1=pos, op=ALU.mult), reads=[A_b, pos_b], writes=[tmp_b])
                    P.op("dve", V("reduce_sum", out=sm[:, 8:9], in_=tmp, axis=AX.X), reads=[tmp_b], writes=[sm_b])
                    P.op("dve", V("tensor_tensor", out=tmp, in0=A[:, k, :], in1=ecap, op=ALU.mult), reads=[A_b, ecap_b], writes=[tmp_b])
                    P.op("dve", V("reduce_sum", out=sm[:, 9:10], in_=tmp, axis=AX.X), reads=[tmp_b], writes=[sm_b])
                    P.op("dve", V("tensor_scalar", out=sm[:, 10:11], in0=sm[:, 8:9], scalar1=float(CAP) - 0.5, scalar2=None,
                                  op0=ALU.is_lt), reads=[sm_b], writes=[sm_b])
                    P.op("dve", V("scalar_tensor_tensor", out=sm[:, 11:12], in0=sm[:, 8:9], scalar=-float(cfg.TRASH), in1=sm[:, 9:10],
                                  op0=ALU.add, op1=ALU.add), reads=[sm_b], writes=[sm_b])
                    P.op("dve", V("tensor_scalar", out=sm[:, 12:13], in0=sm[:, 11:12], scalar1=sm[:, 10:11], scalar2=float(cfg.TRASH),
                                  op0=ALU.mult, op1=ALU.add), reads=[sm_b], writes=[sm_b])
                    rdi, rdi_b = route["di"]
                    P.op("dve", V("tensor_copy", out=rdi[:, t, k:k + 1], in_=sm[:, 12:13]), reads=[sm_b], writes=[rdi_b])
                    ria, ria_b = route["ia"]
                    rib, rib_b = route["ib"]
                    HALF = float(cfg.HALF)
                    P.op("dve", V("tensor_scalar", out=sm[:, 13:14], in0=sm[:, 12:13], scalar1=HALF, scalar2=None, op0=ALU.min),
                         reads=[sm_b], writes=[sm_b])
                    P.op("dve", V("tensor_copy", out=ria[:, t, k:k + 1], in_=sm[:, 13:14]), reads=[sm_b], writes=[ria_b])
                    P.op("dve", V("tensor_scalar", out=sm[:, 14:15], in0=sm[:, 12:13], scalar1=HALF - 0.5, scalar2=None, op0=ALU.is_ge),
                         reads=[sm_b], writes=[sm_b])
                    P.op("dve", V("scalar_tensor_tensor", out=sm[:, 15:16], in0=sm[:, 12:13], scalar=-2.0 * HALF, in1=sm[:, 14:15],
                                  op0=ALU.add, op1=ALU.mult), reads=[sm_b], writes=[sm_b])
                    P.op("dve", V("tensor_scalar", out=sm[:, 15:16], in0=sm[:, 15:16], scalar1=HALF, scalar2=None, op0=ALU.add),
                         reads=[sm_b], writes=[sm_b])
                    P.op("dve", V("tensor_copy", out=rib[:, t, k:k + 1], in_=sm[:, 15:16]), reads=[sm_b], writes=[rib_b])
                    P.op("dve", V("tensor_tensor", out=rw[:, t, k:k + 1], in0=rw[:, t, k:k + 1], in1=sm[:, 10:11], op=ALU.mult),
                         reads=[sm_b, rw_b], writes=[rw_b])
                P.dma("sp", V("dma_start", out=tok, in_=c_tok[t]), writes=[tok_b], grp="ld")
                for k in range(2):
                    P.dma("pool", lambda e, k=k, tok=tok, t=t: e.indirect_dma_start(
                        out=tab_d, out_offset=bass.IndirectOffsetOnAxis(ap=route["di"][0][:, t, k:k + 1], axis=0), in_=tok, in_offset=None),
                        reads=[route["di"][1], tok_b, P.dbuf("tab")], writes=[P.dbuf("tabw")], grp="sc")
        P.release()
        P.barrier()

    def phase_gemm(name, actT, actkey, KC, tiles, nblocks, load_w, epilogue, groups=None, pre_block=None):
        P.mark()
        if groups is None:
            groups = [(0, KC)]
        ng = len(groups)
        wb = []
        for i in range(2):
            w_ap, w_b0 = P.tile([128, KC, 512], BF16, f"{name}_w{i}")
            wb.append((w_ap, [w_b0] + [Buf(f"{name}_w{i}_{g}") for g in range(1, ng)]))
        ab = [P.tile([128, KC, 128], BF16, f"{name}_a{i}") for i in range(3)]
        banks = [[(j * ng + g) for g in range(ng)] for j in range(8 // ng if ng > 1 else 4)]
        nbk = len(banks)
        state = {}
        load_w(0, wb[0])
        it = 0
        for nb in range(nblocks):
            w, w_bs = wb[nb % 2]
            if nb + 1 < nblocks:
                load_w(nb + 1, wb[(nb + 1) % 2])
            if pre_block is not None:
                pre_block(nb, state)
            for t in tiles:
                a, a_b = ab[it % 3]
                P.dma("sp", V("dma_start", out=a, in_=actT[t]), reads=[P.dbuf(actkey, t)], writes=[a_b], grp="ld")
                bks = banks[it % nbk]
                for gi, (k0, k1) in enumerate(groups):
                    pso = P.ps(bks[gi])

                    def mm(e, a=a, w=w, pso=pso, k0=k0, k1=k1):
                        ins = None
                        for k in range(k0, k1):
                            ins = e.matmul(pso[:, :], lhsT=a[:, k, :], rhs=w[:, k, :], start=(k == k0), stop=(k == k1 - 1))
                        return ins
                    P.op("pe", mm, reads=[a_b, w_bs[gi]], writes=[P.psb[bks[gi]]])
                epilogue(nb, t, bks, it, state)
                it += 1
        P.release()
        P.barrier()

    def phase_inproj(l, tiles):
        P.mark()
        zo = [P.tile([128, 512], BF16, f"zo{i}") for i in range(4)]
        zf = [P.tile([128, 512], F32, f"zf{i}") for i in range(2)]
        t1 = [P.tile([128, 512], F32, f"zt{i}") for i in range(2)]
        t2 = [P.tile([128, 256], F32, f"zu{i}") for i in range(2)]
        rp = [P.tile([128, 128], F32, f"rp{i}") for i in range(2)]
        ks = 128.0 ** -0.5

        def kind_of(nb):
            c0 = nb * 512
            for n, (lo, hi) in cfg.cols.items():
                if lo <= c0 < hi:
                    return n
            raise AssertionError

        def load_w(nb, wt):
            w, w_bs = wt
            P.dma("pool", V("dma_start", out=w, in_=w_in[l, :, nb * 512:(nb + 1) * 512].rearrange("(k p) n -> p k n", p=128)),
                  writes=[w_bs[0]], grp="w")

        def epi(nb, t, bks, it, state):
            kind = kind_of(nb)
            bk = bks[0]
            ps = P.ps(bk)
            psb = P.psb[bk]
            o, o_b = zo[it % 4]
            if kind.startswith("gate"):
                P.op("act", V("activation", out=o, in_=ps, func=AF.Sigmoid), reads=[psb], writes=[o_b])
            elif kind in ("gm_u", "gm_v"):
                f, f_b = zf[it % 2]
                u, u_b = t1[it % 2]
                P.op("act", V("activation", out=f, in_=ps, func=AF.Square), reads=[psb], writes=[f_b])
                P.op("dve", V("tensor_scalar", out=f, in0=f, scalar1=0.044715, scalar2=1.0, op0=ALU.mult, op1=ALU.add),
                     reads=[f_b], writes=[f_b])
                P.op("dve", V("tensor_tensor", out=u, in0=f, in1=ps, op=ALU.mult), reads=[f_b, psb], writes=[u_b])
                P.op("act", V("activation", out=f, in_=u, func=AF.Sigmoid, scale=2.0 * math.sqrt(2.0 / math.pi)), reads=[u_b],
                     writes=[f_b])
                P.op("dve", V("tensor_tensor", out=o, in0=f, in1=ps, op=ALU.mult), reads=[f_b, psb], writes=[o_b])
            elif kind == "ret_g":
                P.op("act", V("activation", out=o, in_=ps, func=AF.Silu), reads=[psb], writes=[o_b])
            elif kind == "na_q":
                P.op("act", V("mul", out=o, in_=ps, mul=ks), reads=[psb], writes=[o_b])
            elif kind in ("ret_q", "ret_k") and t < NTL:
                f, f_b = zf[it % 2]
                u, u_b = t1[it % 2]
                v2, v2_b = t2[it % 2]
                r_t, r_b = rp[it % 2]
                P.dma("sp", V("dma_start", out=r_t, in_=c_rope[t]), writes=[r_b], grp="ld")
                P.op("act", V("mul", out=f, in_=ps, mul=(ks if kind == "ret_k" else 1.0)), reads=[psb], writes=[f_b])
                f5 = f.rearrange("p (h a b c) -> p h a b c", h=4, a=2, b=2)
                o5 = o.rearrange("p (h a b c) -> p h a b c", h=4, a=2, b=2)
                u5 = u.rearrange("p (x h a c) -> p x h a c", x=2, h=4, a=2)
                cosv = r_t[:, 0:64].rearrange("p (a c) -> p a c", a=2)
                sinv = r_t[:, 64:128].rearrange("p (a c) -> p a c", a=2)
                for h in range(4):
                    a1 = f5[:, h, :, 0, :]
                    a2 = f5[:, h, :, 1, :]
                    P.op("dve", V("tensor_tensor", out=u5[:, 0, h], in0=a1, in1=cosv, op=ALU.mult), reads=[f_b, r_b], writes=[u_b])
                    P.op("pool", V("tensor_tensor", out=u5[:, 1, h], in0=a2, in1=sinv, op=ALU.mult), reads=[f_b, r_b], writes=[v2_b])
                    P.op("dve", V("tensor_tensor", out=o5[:, h, :, 0, :], in0=u5[:, 0, h], in1=u5[:, 1, h], op=ALU.subtract),
                         reads=[u_b, v2_b], writes=[o_b])
                    P.op("dve", V("tensor_tensor", out=u5[:, 0, h], in0=a2, in1=cosv, op=ALU.mult), reads=[f_b, r_b, o_b], writes=[u_b])
                    P.op("pool", V("tensor_tensor", out=u5[:, 1, h], in0=a1, in1=sinv, op=ALU.mult), reads=[f_b, r_b, o_b], writes=[v2_b])
                    P.op("dve", V("tensor_tensor", out=o5[:, h, :, 1, :], in0=u5[:, 0, h], in1=u5[:, 1, h], op=ALU.add),
                         reads=[u_b, v2_b], writes=[o_b])
            elif kind == "ret_k":
                P.op("act", V("mul", out=o, in_=ps, mul=ks), reads=[psb], writes=[o_b])
            else:
                if it % 2 == 0:
                    P.op("act", V("copy", out=o, in_=ps), reads=[psb], writes=[o_b])
                else:
                    P.op("dve", V("tensor_copy", out=o, in_=ps), reads=[psb], writes=[o_b])
            c0 = nb * 512
            if c0 < cfg.ZM0:
                dst = zg_d[t * 128:(t + 1) * 128, c0:c0 + 512]
                key = ("zg", t)
            else:
                dst = zm_d[t * 128:(t + 1) * 128, c0 - cfg.ZM0:c0 - cfg.ZM0 + 512]
                key = ("zm", t)
            P.dma("sp", V("dma_start", out=dst, in_=o), reads=[o_b], writes=[P.dbuf(*key)], grp="st")

        phase_gemm("inp", hT_d, "hT", KD, tiles, cfg.IN // 512, load_w, epi)
        P.release()

    def phase_gmlp(l, tiles):
        P.mark()
        wsn, wsn_b = P.tile([128, 8, 128], F32, "wsn")
        P.dma("sp", V("dma_start", out=wsn, in_=gm_ws[l].rearrange("g t s -> t g s")), writes=[wsn_b], grp="ld")
        wsb, wsb_b = P.tile([128, 8 * 128], BF16, "wsb")
        P.op("dve", V("tensor_copy", out=wsb, in_=wsn.rearrange("p g s -> p (g s)")), reads=[wsn_b], writes=[wsb_b])
        wsT, wsT_b = P.tile([128, 8, 128], BF16, "wsT")
        transpose_to(wsb, wsb_b, wsT, wsT_b, 8, banks=[0])
        bsT, bsT_b = P.tile([128, 8], F32, "bsT")
        P.dma("sp", V("dma_start", out=bsT, in_=gm_bsT[l]), writes=[bsT_b], grp="ld")
        gng, gng_b = P.tile([128, 1024], F32, "gng")
        P.dma("sp", V("dma_start", out=gng, in_=gm_norm_g[l].partition_broadcast(128)), writes=[gng_b], grp="ld")
        ub = [P.tile([128, 2048], BF16, f"guv{i}") for i in range(2)]
        vf = [P.tile([128, 1024], F32, f"gvf{i}") for i in range(2)]
        vn = [P.tile([128, 1024], BF16, f"gvn{i}") for i in range(2)]
        junk, junk_b = P.tile([128, 1024], BF16, "gjunk")
        stt = [P.tile([128, 8], F32, f"gst{i}") for i in range(2)]
        ao = [P.tile([128, 1024], BF16, f"gao{i}") for i in range(2)]
        aT = [P.tile([128, 8, 128], BF16, f"gaT{i}") for i in range(2)]
        u0, _ = zc("gm_u")
        for i, t in enumerate(tiles):
            uv, uv_b = ub[i % 2]
            v_f, v_fb = vf[i % 2]
            v_n, v_nb = vn[i % 2]
            s, s_b = stt[i % 2]
            a_o, a_ob = ao[i % 2]
            a_T, a_Tb = aT[i % 2]
            P.dma("sp", V("dma_start", out=uv, in_=zm_d[t * 128:(t + 1) * 128, u0:u0 + 2048]), reads=[P.dbuf("zm", t)],
                  writes=[uv_b], grp="ld")
            gv = uv[:, 1024:2048]
            P.op("act", V("copy", out=v_f, in_=gv), reads=[uv_b], writes=[v_fb])
            P.op("dve", V("reduce_sum", out=s[:, 0:1], in_=gv, axis=AX.X), reads=[uv_b], writes=[s_b])
            P.op("act", V("activation", out=junk, in_=gv, func=AF.Square, accum_out=s[:, 1:2]), reads=[uv_b], writes=[junk_b, s_b])
            P.op("dve", V("tensor_scalar", out=s[:, 2:4], in0=s[:, 0:2], scalar1=1.0 / 1024, scalar2=None, op0=ALU.mult),
                 reads=[s_b], writes=[s_b])
            P.op("dve", V("tensor_tensor", out=s[:, 4:5], in0=s[:, 2:3], in1=s[:, 2:3], op=ALU.mult), reads=[s_b], writes=[s_b])
            P.op("dve", V("tensor_tensor", out=s[:, 5:6], in0=s[:, 3:4], in1=s[:, 4:5], op=ALU.subtract), reads=[s_b], writes=[s_b])
            P.op("act", V("activation", out=s[:, 6:7], in_=s[:, 5:6], func=AF.Sqrt, bias=epsb[:, 0:1]), reads=[s_b, epsb_b], writes=[s_b])
            P.op("dve", V("reciprocal", out=s[:, 7:8], in_=s[:, 6:7]), reads=[s_b], writes=[s_b])
            P.op("dve", V("tensor_scalar", out=v_f, in0=v_f, scalar1=s[:, 2:3], scalar2=s[:, 7:8], op0=ALU.subtract, op1=ALU.mult),
                 reads=[v_fb, s_b], writes=[v_fb])
            P.op("pool", V("tensor_tensor", out=v_n, in0=v_f, in1=gng, op=ALU.mult), reads=[v_fb, gng_b], writes=[v_nb])
            for half in range(2):
                bk = 1 + half
                pm = P.ps(bk)

                def mm(e, pm=pm, half=half, v_n=v_n):
                    ins = None
                    for g4 in range(4):
                        g = half * 4 + g4
                        ins = e.matmul(pm[:, g4 * 128:(g4 + 1) * 128], lhsT=wsT[:, g, :], rhs=v_n[:, g * 128:(g + 1) * 128],
                                       start=True, stop=True)
                    return ins
                P.op("pe", mm, reads=[wsT_b, v_nb], writes=[P.psb[bk]])
                for g4 in range(4):
                    g = half * 4 + g4
                    P.op("dve", V("scalar_tensor_tensor", out=a_o[:, g * 128:(g + 1) * 128], in0=pm[:, g4 * 128:(g4 + 1) * 128],
                                  scalar=bsT[:, g:g + 1], in1=uv[:, g * 128:(g + 1) * 128], op0=ALU.add, op1=ALU.mult),
                         reads=[P.psb[bk], bsT_b, uv_b], writes=[a_ob])
            transpose_to(a_o, a_ob, a_T, a_Tb, 8, banks=[3])
            P.dma("sp", V("dma_start", out=abrT_d[t, :, 0:8, :], in_=a_T), reads=[a_Tb], writes=[P.dbuf("abrT", t)], grp="st")
        P.release()
        P.barrier()

    def phase_na(l, need_ctx):
        P.mark()
        q0, _ = zc("na_q")
        k0, _ = zc("na_k")
        v0, _ = zc("na_v")
        qT = [P.tile([128, NT, 128], BF16, f"naqT{i}") for i in range(1)]
        kT = [P.tile([128, NT, 128], BF16, f"nakT{i}") for i in range(1)]
        vv = [P.tile([128, NT, 128], BF16, f"nav{i}") for i in range(1)]
        qk = [P.tile([128, 256], BF16, f"naqk{i}") for i in range(3)]
        bias, bias_b = P.tile([128, NV, NKL], F32, "nabias")
        NK = NKL + L
        sb_ = [P.tile([128, NK], F32, f"nas{i}") for i in range(2)]
        pb_ = [P.tile([128, NK], BF16, f"nap{i}") for i in range(2)]
        pT_ = [P.tile([128, NK // 128, 128], BF16, f"napT{i}") for i in range(2)]
        st_ = [P.tile([128, 4], F32, f"nast{i}") for i in range(2)]
        ob_ = [P.tile([128, 128], BF16, f"nao{i}") for i in range(2)]
        oT_ = [P.tile([128, 1, 128], BF16, f"naoT{i}") for i in range(2)]
        it = 0
        for h in range(8):
            q_T, q_Tb = qT[0]
            k_T, k_Tb = kT[0]
            v_v, v_vb = vv[0]
            P.dma("sp", V("dma_start", out=bias, in_=na_bias[l, h].rearrange("v p k -> p v k")), writes=[bias_b], grp="ld")
            P.dma("sp", V("dma_start", out=v_v, in_=zm_d[:, v0 + h * 128:v0 + (h + 1) * 128].rearrange("(t p) d -> p t d", p=128)),
                  reads=[P.dbuf("zm", t) for t in range(NT)], writes=[v_vb], grp="ld")
            for t in range(NT):
                a, a_b = qk[t % 3]
                P.dma("sp", V("dma_start", out=a[:, 0:128], in_=zm_d[t * 128:(t + 1) * 128, q0 + h * 128:q0 + (h + 1) * 128]),
                      reads=[P.dbuf("zm", t)], writes=[a_b], grp="ld")
                P.dma("sp", V("dma_start", out=a[:, 128:256], in_=zm_d[t * 128:(t + 1) * 128, k0 + h * 128:k0 + (h + 1) * 128]),
                      reads=[P.dbuf("zm", t)], writes=[a_b], grp="ld")
                bk = t % 2
                pst = P.ps(bk, BF16)

                def tr(e, a=a, pst=pst):
                    e.transpose(out=pst[:, 0:128], in_=a[:, 0:128], identity=identb)
                    return e.transpose(out=pst[:, 128:256], in_=a[:, 128:256], identity=identb)
                P.op("pe", tr, reads=[a_b, identb_b], writes=[P.psb[bk]])
                if t % 2 == 0:
                    P.op("act", V("copy", out=q_T[:, t, :], in_=pst[:, 0:128]), reads=[P.psb[bk]], writes=[q_Tb])
                    P.op("act", V("copy", out=k_T[:, t, :], in_=pst[:, 128:256]), reads=[P.psb[bk]], writes=[k_Tb])
                else:
                    P.op("dve", V("tensor_copy", out=q_T[:, t, :], in_=pst[:, 0:128]), reads=[P.psb[bk]], writes=[q_Tb])
                    P.op("dve", V("tensor_copy", out=k_T[:, t, :], in_=pst[:, 128:256]), reads=[P.psb[bk]], writes=[k_Tb])
            qtiles = list(range(NTL)) + (list(range(NTL, NT)) if need_ctx else [])
            for t in qtiles:
                s, s_b = sb_[it % 2]
                p, p_b = pb_[it % 2]
                pT, pT_b = pT_[it % 2]
                stt, stt_b = st_[it % 2]
                o, o_b = ob_[it % 2]
                oT, oT_b = oT_[it % 2]
                lat = t < NTL
                nk = NK if lat else L
                b0 = 2 + 3 * (it % 2)
                if lat:
                    a = tile_a[t] // 2
                    var = tile_var[t]
                    kflat = k_T.rearrange("p t j -> p (t j)")
                    ps0 = P.ps(b0)
                    ps1 = P.ps(b0 + 1)
                    n0 = min(512, NKL)

                    def mms(e, t=t, a=a, ps0=ps0, ps1=ps1, n0=n0, kflat=kflat):
                        ins = e.matmul(ps0[:, 0:n0], lhsT=q_T[:, t, :], rhs=kflat[:, a * 128:a * 128 + n0], start=True, stop=True)
                        if NKL > 512:
                            ins = e.matmul(ps1[:, 0:NKL - 512], lhsT=q_T[:, t, :], rhs=kflat[:, a * 128 + 512:a * 128 + NKL],
                                           start=True, stop=True)
                        ins = e.matmul(ps1[:, 128:128 + L], lhsT=q_T[:, t, :], rhs=kflat[:, NTL * 128:NT * 128], start=True, stop=True)
                        return ins
                    P.op("pe", mms, reads=[q_Tb, k_Tb], writes=[P.psb[b0], P.psb[b0 + 1]])
                    P.op("dve", V("tensor_tensor", out=s[:, 0:n0], in0=ps0[:, 0:n0], in1=bias[:, var, 0:n0], op=ALU.add),
                         reads=[P.psb[b0], bias_b], writes=[s_b])
                    if NKL > 512:
                        P.op("dve", V("tensor_tensor", out=s[:, 512:NKL], in0=ps1[:, 0:NKL - 512], in1=bias[:, var, 512:NKL], op=ALU.add),
                             reads=[P.psb[b0 + 1], bias_b], writes=[s_b])
                    P.op("dve", V("tensor_copy", out=s[:, NKL:NK], in_=ps1[:, 128:128 + L]), reads=[P.psb[b0 + 1]], writes=[s_b])
                else:
                    ps0 = P.ps(b0)
                    kflat = k_T.rearrange("p t j -> p (t j)")

                    def mms(e, t=t, ps0=ps0, kflat=kflat):
                        return e.matmul(ps0[:, 0:L], lhsT=q_T[:, t, :], rhs=kflat[:, NTL * 128:NT * 128], start=True, stop=True)
                    P.op("pe", mms, reads=[q_Tb, k_Tb], writes=[P.psb[b0]])
                    P.op("act", V("copy", out=s[:, 0:L], in_=ps0[:, 0:L]), reads=[P.psb[b0]], writes=[s_b])
                P.op("dve", V("reduce_max", out=stt[:, 0:1], in_=s[:, 0:nk], axis=AX.X), reads=[s_b], writes=[stt_b])
                P.op("dve", V("tensor_scalar", out=stt[:, 1:2], in0=stt[:, 0:1], scalar1=-1.0, scalar2=None, op0=ALU.mult),
                     reads=[stt_b], writes=[stt_b])
                P.op("act", V("activation", out=p[:, 0:nk], in_=s[:, 0:nk], func=AF.Exp, bias=stt[:, 1:2], accum_out=stt[:, 2:3]),
                     reads=[s_b, stt_b], writes=[p_b, stt_b])
                P.op("dve", V("reciprocal", out=stt[:, 3:4], in_=stt[:, 2:3]), reads=[stt_b], writes=[stt_b])
                nkc = nk // 128
                pst = P.ps(b0 + 2, BF16)

                def trp(e, p=p, pst=pst, nkc=nkc):
                    ins = None
                    for c in range(nkc):
                        ins = e.transpose(out=pst[:, c * 128:(c + 1) * 128], in_=p[:, c * 128:(c + 1) * 128], identity=identb)
                    return ins
                P.op("pe", trp, reads=[p_b, identb_b], writes=[P.psb[b0 + 2]])
                P.op("dve", V("tensor_copy", out=pT[:, 0:nkc, :].rearrange("p a b -> p (a b)"), in_=pst[:, 0:nk]),
                     reads=[P.psb[b0 + 2]], writes=[pT_b])
                pso = P.ps(b0)
                if lat:
                    vt = [tile_a[t] // 2 + c for c in range(WT)] + list(range(NTL, NT))
                else:
                    vt = list(range(NTL, NT))

                def mmo(e, pT=pT, pso=pso, vt=vt):
                    ins = None
                    for c, tv in enumerate(vt):
                        ins = e.matmul(pso[:, 0:128], lhsT=pT[:, c, :], rhs=v_v[:, tv, :], start=(c == 0), stop=(c == len(vt) - 1))
                    return ins
                P.op("pe", mmo, reads=[pT_b, v_vb], writes=[P.psb[b0]])
                P.op("dve", V("tensor_scalar", out=o, in0=pso[:, 0:128], scalar1=stt[:, 3:4], scalar2=None, op0=ALU.mult),
                     reads=[P.psb[b0], stt_b], writes=[o_b])
                transpose_to(o, o_b, oT, oT_b, 1, banks=[b0 + 2], evac=("act",))
                P.dma("sp", V("dma_start", out=abrT_d[t, :, 8 + h:9 + h, :], in_=oT), reads=[oT_b], writes=[P.dbuf("abrT", t)], grp="st")
                it += 1
        P.release()
        P.barrier()

    def phase_ret(l, need_ctx):
        P.mark()
        q0, _ = zc("ret_q")
        k0, _ = zc("ret_k")
        v0, _ = zc("ret_v")
        g0, _ = zc("ret_g")
        C = 128
        dec, dec_bb = P.tile([128, 16], F32, "dec")
        P.dma("sp", V("dma_start", out=dec[:, 0:8], in_=dec_f[l].partition_broadcast(128)), writes=[dec_bb], grp="ld")
        P.dma("sp", V("dma_start", out=dec[:, 8:16], in_=dec_b[l].partition_broadcast(128)), writes=[dec_bb], grp="ld")
        lg, lg_b = P.tile([128, 16], F32, "lgd")
        one, one_b = P.tile([128, 1], F32, "one")
        P.op("dve", V("memset", one, 1.0), writes=[one_b])
        P.op("act", V("activation", out=lg, in_=dec, func=AF.Exp, scale=math.log(2.0)), reads=[dec_bb], writes=[lg_b])
        P.op("act", V("activation", out=lg, in_=lg, func=AF.Ln, scale=-1.0, bias=one[:, 0:1]), reads=[lg_b, one_b], writes=[lg_b])
        rel, rel_b = P.tile([128, 4, 128], F32, "rel")
        P.dma("sp", V("dma_start", out=rel, in_=c_rel), writes=[rel_b], grp="ld")
        idx, idx_b = P.tile([128, 4, 128], F32, "idx")
        P.dma("sp", V("dma_start", out=idx, in_=c_idx), writes=[idx_b], grp="ld")
        DM, DM_b = P.tile([128, 8, 128], F32, "DM")
        QD, QD_b = P.tile([128, 2, 8, 128], F32, "QD")
        KDc, KDc_b = P.tile([128, 2, 8], F32, "KDc")
        KDx, KDx_b = P.tile([128, 2, 8, 128], BF16, "KDx")
        CD, CD_b = P.tile([128, 16], F32, "CD")
        tmpm, tmpm_b = P.tile([128, 128], F32, "tmpm")
        for h in range(8):
            P.op("act", V("activation", out=DM[:, h, :], in_=rel[:, 0, :], func=AF.Exp, scale=lg[:, h:h + 1]), reads=[rel_b, lg_b],
                 writes=[DM_b])
            P.op("dve", V("tensor_tensor", out=DM[:, h, :], in0=DM[:, h, :], in1=rel[:, 2, :], op=ALU.mult), reads=[DM_b, rel_b],
                 writes=[DM_b])
            P.op("act", V("activation", out=tmpm, in_=rel[:, 1, :], func=AF.Exp, scale=lg[:, 8 + h:9 + h]), reads=[rel_b, lg_b],
                 writes=[tmpm_b])
            P.op("dve", V("tensor_tensor", out=tmpm, in0=tmpm, in1=rel[:, 3, :], op=ALU.mult), reads=[tmpm_b, rel_b], writes=[tmpm_b])
            P.op("dve", V("tensor_tensor", out=DM[:, h, :], in0=DM[:, h, :], in1=tmpm, op=ALU.add), reads=[DM_b, tmpm_b], writes=[DM_b])
            P.op("act", V("activation", out=QD[:, 0, h, :], in_=idx[:, 0, :], func=AF.Exp, scale=lg[:, h:h + 1]), reads=[idx_b, lg_b],
                 writes=[QD_b])
            P.op("act", V("activation", out=QD[:, 1, h, :], in_=idx[:, 1, :], func=AF.Exp, scale=lg[:, 8 + h:9 + h]), reads=[idx_b, lg_b],
                 writes=[QD_b])
            P.op("act", V("activation", out=KDx[:, 0, h, :], in_=idx[:, 2, :], func=AF.Exp, scale=lg[:, h:h + 1]),
                 reads=[idx_b, lg_b], writes=[KDx_b])
            P.op("act", V("activation", out=KDx[:, 1, h, :], in_=idx[:, 3, :], func=AF.Exp, scale=lg[:, 8 + h:9 + h]),
                 reads=[idx_b, lg_b], writes=[KDx_b])
        P.op("act", V("activation", out=CD, in_=lg, func=AF.Exp, scale=float(C)), reads=[lg_b], writes=[CD_b])

        Sf, Sf_b = P.tile([128, 8, 256], F32, "Sf")
        Sb, Sb_b = P.tile([128, 8, 256], F32, "Sb")
        Sfb, Sfb_b = P.tile([128, 8, 256], BF16, "Sfb")
        Sbb = [P.tile([128, 8, 256], BF16, f"Sbb{i}") for i in range(2)]
        kin = [P.tile([128, 1024], BF16, f"rk{i}") for i in range(2)]
        qin = [P.tile([128, 1024], BF16, f"rq{i}") for i in range(2)]
        vin = [P.tile([128, 2048], BF16, f"rv{i}") for i in range(2)]
        gin = [P.tile([128, 2048], BF16, f"rg{i}") for i in range(2)]
        kd = [P.tile([128, 1024], BF16, f"rkd{i}") for i in range(2)]
        qTt = [P.tile([128, 4, 128], BF16, f"rqT{i}") for i in range(2)]
        kTt = [P.tile([128, 4, 128], BF16, f"rkT{i}") for i in range(2)]
        qdf = [P.tile([128, 4, 128], BF16, f"rqdf{i}") for i in range(2)]
        qdb = [P.tile([128, 4, 128], BF16, f"rqdb{i}") for i in range(2)]
        Mt = [P.tile([128, 4, 128], BF16, f"rMt{i}") for i in range(2)]
        osb = [P.tile([128, 4, 256], F32, f"ros{i}") for i in range(2)]
        osq, osq_b = P.tile([128, 4, 256], BF16, "rosq")
        hst = [P.tile([128, 6, 4], F32, f"rhs{i}") for i in range(2)]
        rr = [P.tile([128, 2048], BF16, f"rrr{i}") for i in range(2)]
        rT = [P.tile([128, 16, 128], BF16, f"rrT{i}") for i in range(2)]

        P.op("dve", V("memset", Sf.rearrange("p a b -> p (a b)"), 0.0), writes=[Sf_b])
        P.op("pool", V("memset", Sb.rearrange("p a b -> p (a b)"), 0.0), writes=[Sb_b])
        cnt = {"ld": 0}

        def load_kv(t, need_q):
            i = cnt["ld"]
            cnt["ld"] += 1
            k_t, k_b = kin[i % 2]
            v_t, v_b = vin[i % 2]
            rows = slice(t * 128, (t + 1) * 128)
            P.dma("sp", V("dma_start", out=k_t, in_=zm_d[rows, k0:k0 + 1024]), reads=[P.dbuf("zm", t)], writes=[k_b], grp="ld")
            P.dma("sp", V("dma_start", out=v_t, in_=zm_d[rows, v0:v0 + 2048]), reads=[P.dbuf("zm", t)], writes=[v_b], grp="ld")
            if need_q:
                q_t, q_b = qin[i % 2]
                g_t, g_b = gin[i % 2]
                P.dma("sp", V("dma_start", out=q_t, in_=zm_d[rows, q0:q0 + 1024]), reads=[P.dbuf("zm", t)], writes=[q_b], grp="ld")
                P.dma("sp", V("dma_start", out=g_t, in_=zm_d[rows, g0:g0 + 2048]), reads=[P.dbuf("zm", t)], writes=[g_b], grp="ld")
            return i

        def state_update(i, S, S_b, d, banks):
            k_t, k_b = kin[i % 2]
            v_t, v_b = vin[i % 2]
            kd_t, kd_b = kd[i % 2]
            P.op("pool", V("tensor_tensor", out=kd_t, in0=k_t, in1=KDx[:, d].rearrange("p h d -> p (h d)"), op=ALU.mult),
                 reads=[k_b, KDx_b], writes=[kd_b])
            for hh in range(4):
                bk = banks[hh % len(banks)]
                pu = P.ps(bk)

                def mmu(e, hh=hh, pu=pu, kd_t=kd_t, v_t=v_t):
                    ins = None
                    for j in range(2):
                        h = hh * 2 + j
                        ins = e.matmul(pu[:, j * 256:(j + 1) * 256], lhsT=kd_t[:, h * 128:(h + 1) * 128],
                                       rhs=v_t[:, h * 256:(h + 1) * 256], start=True, stop=True)
                    return ins
                P.op("pe", mmu, reads=[kd_b, v_b], writes=[P.psb[bk]])
                for j in range(2):
                    h = hh * 2 + j
                    P.op("dve", V("scalar_tensor_tensor", out=S[:, h, :], in0=S[:, h, :], scalar=CD[:, 8 * d + h:8 * d + h + 1],
                                  in1=pu[:, j * 256:(j + 1) * 256], op0=ALU.mult, op1=ALU.add),
                         reads=[S_b, CD_b, P.psb[bk]], writes=[S_b])

        def backward_pass(tiles):
            for t in reversed(tiles):
                i = load_kv(t, False)
                sbb, sbb_b = Sbb[i % 2]
                P.op("act", V("copy", out=sbb.rearrange("p a b -> p (a b)"), in_=Sb.rearrange("p a b -> p (a b)")), reads=[Sb_b],
                     writes=[sbb_b])
                P.dma("sp", V("dma_start", out=sb_d[t], in_=sbb), reads=[sbb_b], writes=[P.dbuf("sb", t)], grp="st")
                state_update(i, Sb, Sb_b, 1, banks=[0, 1])

        def forward_pass(tiles, emit_out):
            for t in tiles:
                i = load_kv(t, emit_out)
                if emit_out:
                    k_t, k_b = kin[i % 2]
                    q_t, q_b = qin[i % 2]
                    v_t, v_b = vin[i % 2]
                    g_t, g_b = gin[i % 2]
                    sbb, sbb_b = Sbb[i % 2]
                    P.dma("sp", V("dma_start", out=sbb, in_=sb_d[t]), reads=[P.dbuf("sb", t)], writes=[sbb_b], grp="ld")
                    P.op("act", V("copy", out=Sfb.rearrange("p a b -> p (a b)"), in_=Sf.rearrange("p a b -> p (a b)")), reads=[Sf_b],
                         writes=[Sfb_b])
                    r_t, r_b = rr[i % 2]
                    r_T, r_Tb = rT[i % 2]
                    for half in range(2):
                        j2 = 2 * i + half
                        q_T, q_Tb = qTt[j2 % 2]
                        k_T, k_Tb = kTt[j2 % 2]
                        q_f, q_fb = qdf[j2 % 2]
                        q_bw, q_bwb = qdb[j2 % 2]
                        M_t, M_b = Mt[j2 % 2]
                        o_s, o_sb = osb[j2 % 2]
                        hs, hs_b = hst[j2 % 2]
                        hsl = slice(half * 4, half * 4 + 4)
                        pq = P.ps(2, BF16)

                        def trq(e, pq=pq, q_t=q_t, k_t=k_t, half=half):
                            ins = None
                            for hh in range(4):
                                h = half * 4 + hh
                                e.transpose(out=pq[:, hh * 128:(hh + 1) * 128], in_=q_t[:, h * 128:(h + 1) * 128], identity=identb)
                                ins = e.transpose(out=pq[:, 512 + hh * 128:512 + (hh + 1) * 128], in_=k_t[:, h * 128:(h + 1) * 128],
                                                  identity=identb)
                            return ins
                        P.op("pe", trq, reads=[q_b, k_b, identb_b], writes=[P.psb[2]])
                        P.op("act", V("copy", out=q_T.rearrange("p a b -> p (a b)"), in_=pq[:, 0:512]), reads=[P.psb[2]], writes=[q_Tb])
                        P.op("act", V("copy", out=k_T.rearrange("p a b -> p (a b)"), in_=pq[:, 512:1024]), reads=[P.psb[2]], writes=[k_Tb])
                        P.op("dve", V("tensor_tensor", out=q_f, in0=q_T, in1=QD[:, 0, hsl, :],
                                      op=ALU.mult), reads=[q_Tb, QD_b], writes=[q_fb])
                        P.op("pool", V("tensor_tensor", out=q_bw, in0=q_T, in1=QD[:, 1, hsl, :],
                                      op=ALU.mult), reads=[q_Tb, QD_b], writes=[q_bwb])
                        pp = P.ps(3)

                        def mmp(e, pp=pp, k_T=k_T, q_T=q_T):
                            ins = None
                            for hh in range(4):
                                ins = e.matmul(pp[:, hh * 128:(hh + 1) * 128], lhsT=k_T[:, hh, :], rhs=q_T[:, hh, :], start=True, stop=True)
                            return ins
                        P.op("pe", mmp, reads=[k_Tb, q_Tb], writes=[P.psb[3]])
                        P.op("dve", V("tensor_tensor", out=M_t, in0=pp.rearrange("p (a b) -> p a b", a=4), in1=DM[:, hsl, :], op=ALU.mult),
                             reads=[P.psb[3], DM_b], writes=[M_b])
                        for pr in range(2):
                            bk = 4 + pr
                            po = P.ps(bk)

                            def mmo(e, po=po, pr=pr, half=half, M_t=M_t, q_f=q_f, q_bw=q_bw, v_t=v_t, sbb=sbb):
                                ins = None
                                for j in range(2):
                                    hh = pr * 2 + j
                                    h = half * 4 + hh
                                    oo = po[:, j * 256:(j + 1) * 256]
                                    e.matmul(oo, lhsT=M_t[:, hh, :], rhs=v_t[:, h * 256:(h + 1) * 256], start=True, stop=False)
                                    e.matmul(oo, lhsT=q_f[:, hh, :], rhs=Sfb[:, h, :], start=False, stop=False)
                                    ins = e.matmul(oo, lhsT=q_bw[:, hh, :], rhs=sbb[:, h, :], start=False, stop=True)
                                return ins
                            P.op("pe", mmo, reads=[M_b, q_fb, q_bwb, v_b, Sfb_b, sbb_b], writes=[P.psb[bk]])
                            P.op("act", V("copy", out=o_s[:, pr * 2:pr * 2 + 2, :].rearrange("p a b -> p (a b)"), in_=po), reads=[P.psb[bk]],
                                 writes=[o_sb])
                        P.op("dve", V("reduce_sum", out=hs[:, 0, :], in_=o_s, axis=AX.X), reads=[o_sb], writes=[hs_b])
                        P.op("act", V("activation", out=osq.rearrange("p a b -> p (a b)"), in_=o_s.rearrange("p a b -> p (a b)"),
                                      func=AF.Square), reads=[o_sb], writes=[osq_b])
                        P.op("dve", V("reduce_sum", out=hs[:, 1, :], in_=osq, axis=AX.X), reads=[osq_b], writes=[hs_b])
                        P.op("dve", V("tensor_scalar", out=hs[:, 0:2, :].rearrange("p a b -> p (a b)"),
                                      in0=hs[:, 0:2, :].rearrange("p a b -> p (a b)"), scalar1=1.0 / 256, scalar2=None, op0=ALU.mult),
                             reads=[hs_b], writes=[hs_b])
                        P.op("dve", V("tensor_tensor", out=hs[:, 2, :], in0=hs[:, 0, :], in1=hs[:, 0, :], op=ALU.mult), reads=[hs_b],
                             writes=[hs_b])
                        P.op("dve", V("tensor_tensor", out=hs[:, 3, :], in0=hs[:, 1, :], in1=hs[:, 2, :], op=ALU.subtract), reads=[hs_b],
                             writes=[hs_b])
                        P.op("act", V("activation", out=hs[:, 4, :], in_=hs[:, 3, :], func=AF.Sqrt, bias=epsb[:, 0:1]), reads=[hs_b, epsb_b],
                             writes=[hs_b])
                        P.op("dve", V("reciprocal", out=hs[:, 5, :], in_=hs[:, 4, :]), reads=[hs_b], writes=[hs_b])
                        for hh in range(4):
                            P.op("dve", V("tensor_scalar", out=o_s[:, hh, :], in0=o_s[:, hh, :], scalar1=hs[:, 0, hh:hh + 1],
                                          scalar2=hs[:, 5, hh:hh + 1], op0=ALU.subtract, op1=ALU.mult), reads=[o_sb, hs_b], writes=[o_sb])
                        P.op("pool", V("tensor_tensor", out=r_t[:, half * 1024:(half + 1) * 1024], in0=o_s.rearrange("p a b -> p (a b)"),
                                       in1=g_t[:, half * 1024:(half + 1) * 1024], op=ALU.mult), reads=[o_sb, g_b], writes=[r_b])
                    transpose_to(r_t, r_b, r_T, r_Tb, 16, banks=[0, 1])
                    P.dma("sp", V("dma_start", out=abrT_d[t, :, 16:32, :], in_=r_T), reads=[r_Tb], writes=[P.dbuf("abrT", t)], grp="st")
                state_update(i, Sf, Sf_b, 0, banks=[6, 7])

        lat = list(range(NTL))
        ctxt = list(range(NTL, NT))
        backward_pass(ctxt)
        forward_pass(ctxt, need_ctx)
        backward_pass(lat)
        forward_pass(lat, True)
        P.release()
        P.barrier()

    def phase_merge(l, tiles):
        P.mark()
        gt = [P.tile([128, 3, 512], BF16, f"mg{i}") for i in range(2)]
        y1 = [P.tile([128, 512], F32, f"my1{i}") for i in range(2)]
        y2 = [P.tile([128, 512], F32, f"my2{i}") for i in range(2)]
        y3 = [P.tile([128, 512], F32, f"my3{i}") for i in range(2)]
        yb = [P.tile([128, 512], BF16, f"myb{i}") for i in range(2)]
        yT = [P.tile([128, 4, 128], BF16, f"myT{i}") for i in range(2)]

        def load_w(nb, wt):
            w, w_bs = wt
            cs = slice(nb * 512, (nb + 1) * 512)
            P.dma("pool", V("dma_start", out=w[:, 0:8, :], in_=w_a[l, :, cs].rearrange("(k p) n -> p k n", p=128)), writes=[w_bs[0]], grp="w")
            P.dma("pool", V("dma_start", out=w[:, 8:16, :], in_=w_b[l, :, cs].rearrange("(k p) n -> p k n", p=128)), writes=[w_bs[1]], grp="w")
            P.dma("pool", V("dma_start", out=w[:, 16:32, :], in_=w_c[l, :, cs].rearrange("(k p) n -> p k n", p=128)), writes=[w_bs[2]], grp="w")

        def epi(nb, t, bks, it, state):
            g, g_b = gt[it % 2]
            a1, a1_b = y1[it % 2]
            a2, a2_b = y2[it % 2]
            a3, a3_b = y3[it % 2]
            b, b_b = yb[it % 2]
            bT, bT_b = yT[it % 2]
            for j in range(3):
                P.dma("sp", V("dma_start", out=g[:, j, :], in_=zg_d[t * 128:(t + 1) * 128, j * D + nb * 512:j * D + (nb + 1) * 512]),
                      reads=[P.dbuf("zg", t)], writes=[g_b], grp="ld")
            P.op("dve", V("tensor_tensor", out=a1, in0=P.ps(bks[0]), in1=g[:, 0, :], op=ALU.mult), reads=[P.psb[bks[0]], g_b], writes=[a1_b])
            P.op("dve", V("tensor_tensor", out=a2, in0=P.ps(bks[1]), in1=g[:, 1, :], op=ALU.mult), reads=[P.psb[bks[1]], g_b], writes=[a2_b])
            P.op("dve", V("tensor_tensor", out=a3, in0=P.ps(bks[2]), in1=g[:, 2, :], op=ALU.mult), reads=[P.psb[bks[2]], g_b], writes=[a3_b])
            P.op("pool", V("tensor_tensor", out=a1, in0=a1, in1=a2, op=ALU.add), reads=[a1_b, a2_b], writes=[a1_b])
            P.op("pool", V("tensor_tensor", out=b, in0=a1, in1=a3, op=ALU.add), reads=[a1_b, a3_b], writes=[b_b])
            transpose_to(b, b_b, bT, bT_b, 4, banks=[6, 7], evac=("act",))
            P.dma("sp", V("dma_start", out=yT_d[t, :, nb * 4:(nb + 1) * 4, :], in_=bT), reads=[bT_b], writes=[P.dbuf("yT", t)], grp="st")

        phase_gemm("mrg", abrT_d, "abrT", 32, tiles, NBD, load_w, epi, groups=[(0, 8), (8, 16), (16, 32)])
        P.release()

    def phase_outproj(l, tiles):
        P.mark()
        g1 = [P.tile([128, 2, 512], F32, f"og{i}") for i in range(2)]
        xs = [P.tile([128, 512], F32, f"ox{i}") for i in range(3)]
        tm = [P.tile([128, 512], F32, f"ot{i}") for i in range(2)]

        def load_w(nb, wt):
            w, w_bs = wt
            P.dma("pool", V("dma_start", out=w, in_=w_out[l, :, nb * 512:(nb + 1) * 512].rearrange("(k p) n -> p k n", p=128)),
                  writes=[w_bs[0]], grp="w")

        def pre(nb, state):
            g, g_b = g1[nb % 2]
            for r in range(2):
                P.dma("sp", V("dma_start", out=g[:, r, :], in_=modd[l, r, 2 * D + nb * 512:2 * D + (nb + 1) * 512].partition_broadcast(128)),
                      reads=[P.dbuf("modd")], writes=[g_b], grp="ld")
            state["g"] = (g, g_b)

        def epi(nb, t, bks, it, state):
            g, g_b = state["g"]
            r = 0 if t < NTL else 1
            x_t, x_b = xs[it % 3]
            m, m_b = tm[it % 2]
            P.dma("sp", V("dma_start", out=x_t, in_=xres[t * 128:(t + 1) * 128, nb * 512:(nb + 1) * 512]),
                  reads=[P.dbuf("xres", "all"), P.dbuf("xres", t)], writes=[x_b], grp="ld")
            P.op("dve", V("tensor_tensor", out=m, in0=P.ps(bks[0]), in1=g[:, r, :], op=ALU.mult), reads=[P.psb[bks[0]], g_b], writes=[m_b])
            P.op("pool", V("tensor_tensor", out=x_t, in0=x_t, in1=m, op=ALU.add), reads=[x_b, m_b], writes=[x_b])
            P.dma("sp", V("dma_start", out=xres[t * 128:(t + 1) * 128, nb * 512:(nb + 1) * 512], in_=x_t), reads=[x_b],
                  writes=[P.dbuf("xres", t)], grp="st")

        phase_gemm("out", yT_d, "yT", KD, tiles, NBD, load_w, epi, pre_block=pre)
        P.release()

    def phase_moe(l):
        P.mark()
        WSZ = KD * MH
        ring = [P.tile([128, WSZ], BF16, f"mw{i}") for i in range(4)]
        idxb = [P.tile([128, 4], I32, f"mi{i}") for i in range(2)]
        xbb = [P.tile([128, D], BF16, f"mx{i}") for i in range(2)]
        xT = [P.tile([128, KD, 128], BF16, f"mxT{i}") for i in range(2)]
        s1 = [P.tile([128, MH], F32, f"ms{i}") for i in range(2)]
        ab = [P.tile([128, MH], BF16, f"ma{i}") for i in range(2)]
        aT = [P.tile([128, KH, 128], BF16, f"maT{i}") for i in range(2)]
        yb = [P.tile([128, D], BF16, f"my{i}") for i in range(2)]
        rc = 0
        it = 0
        pend = {}

        def load_e(e):
            nonlocal rc
            res = []
            for piece, src in ((0, moe_w1[l, e]), (1, moe_w3[l, e]), (2, moe_w2[l, e])):
                w, w_b = ring[rc % 4]
                rc += 1
                if piece < 2:
                    P.dma("pool", V("dma_start", out=w.rearrange("p (k n) -> p k n", k=KD), in_=src.rearrange("(k p) n -> p k n", p=128)),
                          writes=[w_b], grp="w")
                else:
                    P.dma("pool", V("dma_start", out=w.rearrange("p (k n) -> p k n", k=KH), in_=src.rearrange("(k p) n -> p k n", p=128)),
                          writes=[w_b], grp="w")
                res.append((w, w_b))
            return res

        for e in range(NE):
            (w1, w1_b), (w3, w3_b), (w2, w2_b) = load_e(e)
            w1v = w1.rearrange("p (k n) -> p k n", k=KD)
            w3v = w3.rearrange("p (k n) -> p k n", k=KD)
            w2v = w2.rearrange("p (k n) -> p k n", k=KH)
            for j in range(CAP // 128):
                ix, ix_b = idxb[it % 2]
                x_t, x_b = xbb[it % 2]
                x_T, x_Tb = xT[it % 2]
                s_t, s_b = s1[it % 2]
                a_t, a_b = ab[it % 2]
                a_T, a_Tb = aT[it % 2]
                y_t, y_b = yb[it % 2]
                r0 = e * CAP + j * 128
                P.dma("sp", V("dma_start", out=ix, in_=tab_d[r0:r0 + 128, :]), reads=[P.dbuf("tabw"), P.dbuf("tab")], writes=[ix_b], grp="ld")
                P.dma("pool", lambda en, x_t=x_t, ix=ix: en.indirect_dma_start(
                    out=x_t, out_offset=None, in_=h2_d, in_offset=bass.IndirectOffsetOnAxis(ap=ix[:, 0:1], axis=0)),
                    reads=[ix_b], writes=[x_b], grp="ga")
                transpose_to(x_t, x_b, x_T, x_Tb, KD, banks=[0, 1])
                p1 = P.ps(2)
                p3 = P.ps(3)

                def mm13(en, p1=p1, p3=p3, x_T=x_T, w1v=w1v, w3v=w3v):
                    ins = None
                    for k in range(KD):
                        en.matmul(p1[:, 0:MH], lhsT=x_T[:, k, :], rhs=w1v[:, k, :], start=(k == 0), stop=(k == KD - 1))
                    for k in range(KD):
                        ins = en.matmul(p3[:, 0:MH], lhsT=x_T[:, k, :], rhs=w3v[:, k, :], start=(k == 0), stop=(k == KD - 1))
                    return ins
                P.op("pe", mm13, reads=[x_Tb, w1_b, w3_b], writes=[P.psb[2], P.psb[3]])
                P.op("act", V("activation", out=s_t, in_=p1[:, 0:MH], func=AF.Silu), reads=[P.psb[2]], writes=[s_b])
                P.op("dve", V("tensor_tensor", out=a_t, in0=s_t, in1=p3[:, 0:MH], op=ALU.mult), reads=[s_b, P.psb[3]], writes=[a_b])
                transpose_to(a_t, a_b, a_T, a_Tb, KH, banks=[4], evac=("act",))
                for nb2 in range(NBD):
                    bk = 5 + nb2 % 3
                    py = P.ps(bk)

                    def mmy(en, py=py, a_T=a_T, w2v=w2v, nb2=nb2):
                        ins = None
                        for k in range(KH):
                            ins = en.matmul(py, lhsT=a_T[:, k, :], rhs=w2v[:, k, nb2 * 512:(nb2 + 1) * 512], start=(k == 0), stop=(k == KH - 1))
                        return ins
                    P.op("pe", mmy, reads=[a_Tb, w2_b], writes=[P.psb[bk]])
                    if nb2 % 2 == 0:
                        P.op("act", V("copy", out=y_t[:, nb2 * 512:(nb2 + 1) * 512], in_=py), reads=[P.psb[bk]], writes=[y_b])
                    else:
                        P.op("dve", V("tensor_copy", out=y_t[:, nb2 * 512:(nb2 + 1) * 512], in_=py), reads=[P.psb[bk]], writes=[y_b])
                if e < NE // 2:
                    ydst = ys_a[r0:r0 + 128, :]
                else:
                    ydst = ys_b[r0 - cfg.HALF:r0 - cfg.HALF + 128, :]
                P.dma("sp", V("dma_start", out=ydst, in_=y_t), reads=[y_b], writes=[P.dbuf("ys", it % 8)], grp="st")
                it += 1
        P.release()
        P.barrier()

    def phase_final(l, tiles, route, last):
        P.mark()
        g2 = []
        for r in range(2):
            g_t, g_b = P.tile([128, D], F32, f"fg2{r}")
            P.dma("sp", V("dma_start", out=g_t, in_=modrow(l, r, 5).partition_broadcast(128)), reads=[P.dbuf("modd")], writes=[g_b], grp="ld")
            g2.append((g_t, g_b))
        if last:
            fg, fg_b = P.tile([128, D], F32, "ffg")
            P.dma("sp", V("dma_start", out=fg, in_=final_g.partition_broadcast(128)), writes=[fg_b], grp="ld")
            junk, junk_b = P.tile([128, D], BF16, "fjunk")
        yy = [P.tile([128, 4, D], BF16, f"fy{i}") for i in range(2)]
        xx = [P.tile([128, D], F32, f"fx{i}") for i in range(2)]
        ff = [P.tile([128, D], F32, f"ff{i}") for i in range(2)]
        sq_ = [P.tile([128, 4], F32, f"fs{i}") for i in range(2)]
        rw, rw_b = route["w"]
        ria, ria_b = route["ia"]
        rib, rib_b = route["ib"]
        for i, t in enumerate(tiles):
            r = 0 if t < NTL else 1
            g_t, g_b = g2[r]
            y_t, y_b = yy[i % 2]
            x_t, x_b = xx[i % 2]
            f_t, f_b = ff[i % 2]
            for k in range(2):
                P.dma("pool", lambda en, y_t=y_t, k=k, t=t: en.indirect_dma_start(
                    out=y_t[:, 2 * k, :], out_offset=None, in_=ys_a, in_offset=bass.IndirectOffsetOnAxis(ap=ria[:, t, k:k + 1], axis=0)),
                    reads=[ria_b], writes=[y_b], grp="ga")
                P.dma("pool", lambda en, y_t=y_t, k=k, t=t: en.indirect_dma_start(
                    out=y_t[:, 2 * k + 1, :], out_offset=None, in_=ys_b, in_offset=bass.IndirectOffsetOnAxis(ap=rib[:, t, k:k + 1], axis=0)),
                    reads=[rib_b], writes=[y_b], grp="ga")
            P.dma("sp", V("dma_start", out=x_t, in_=xres[t * 128:(t + 1) * 128, :]), reads=[P.dbuf("xres", "all"), P.dbuf("xres", t)],
                  writes=[x_b], grp="ld")
            P.op("dve", V("tensor_scalar", out=f_t, in0=y_t[:, 0, :], scalar1=rw[:, t, 0:1], scalar2=None, op0=ALU.mult),
                 reads=[y_b, rw_b], writes=[f_b])
            for jj, kk in ((1, 0), (2, 1), (3, 1)):
                P.op("dve", V("scalar_tensor_tensor", out=f_t, in0=y_t[:, jj, :], scalar=rw[:, t, kk:kk + 1], in1=f_t, op0=ALU.mult,
                              op1=ALU.add), reads=[y_b, rw_b, f_b], writes=[f_b])
            P.op("pool", V("tensor_tensor", out=f_t, in0=f_t, in1=g_t, op=ALU.mult), reads=[f_b, g_b], writes=[f_b])
            P.op("pool", V("tensor_tensor", out=x_t, in0=x_t, in1=f_t, op=ALU.add), reads=[x_b, f_b], writes=[x_b])
            if not last:
                P.dma("sp", V("dma_start", out=xres[t * 128:(t + 1) * 128, :], in_=x_t), reads=[x_b], writes=[P.dbuf("xres", t)], grp="st")
            else:
                sq, sq_b = sq_[i % 2]
                P.op("act", V("activation", out=junk, in_=x_t, func=AF.Square, accum_out=sq[:, 0:1]), reads=[x_b], writes=[junk_b, sq_b])
                P.op("act", V("activation", out=sq[:, 1:2], in_=sq[:, 0:1], func=AF.Sqrt, scale=1.0 / D, bias=epsb[:, 0:1]),
                     reads=[sq_b, epsb_b], writes=[sq_b])
                P.op("dve", V("reciprocal", out=sq[:, 2:3], in_=sq[:, 1:2]), reads=[sq_b], writes=[sq_b])
                P.op("dve", V("scalar_tensor_tensor", out=x_t, in0=x_t, scalar=sq[:, 2:3], in1=fg, op0=ALU.mult, op1=ALU.mult),
                     reads=[x_b, sq_b, fg_b], writes=[x_b])
                P.dma("sp", V("dma_start", out=out_d[t * 128:(t + 1) * 128, :], in_=x_t), reads=[x_b], writes=[P.dbuf("out")], grp="st")
        P.release()
        P.barrier()

    def dump(src_ap):
        P.barrier()
        P.dma("sp", V("dma_start", out=dbg_d, in_=src_ap), writes=[P.dbuf("dbg")], grp="st")

    stages = ["norm1", "inproj", "gmlp", "na", "ret", "merge", "outproj", "norm2", "moe", "final"]

    def done(l, s):
        return stop_after is not None and stop_after == (l, s)

    finished = False
    for l in range(depth):
        need_ctx = l < depth - 1
        tiles_all = list(range(NT))
        act_tiles = tiles_all if need_ctx else list(range(NTL))
        seq = [
            ("norm1", lambda: phase_norm(l, 0, tiles_all)),
            ("inproj", lambda: phase_inproj(l, tiles_all)),
            ("gmlp", lambda: phase_gmlp(l, act_tiles)),
            ("na", lambda: phase_na(l, need_ctx)),
            ("ret", lambda: phase_ret(l, need_ctx)),
            ("merge", lambda: phase_merge(l, act_tiles)),
            ("outproj", lambda: phase_outproj(l, act_tiles)),
        ]
        for s, fn in seq:
            fn()
            if done(l, s):
                finished = True
                break
        if finished:
            break
        P.mark()
        route = {"w": P.tile([128, NT, 2], F32, "route_w"), "d": P.tile([128, NT, 2], F32, "route_d"),
                 "di": P.tile([128, NT, 2], I32, "route_di"), "ia": P.tile([128, NT, 2], I32, "route_ia"),
                 "ib": P.tile([128, NT, 2], I32, "route_ib")}
        phase_norm(l, 1, act_tiles, route)
        if done(l, "norm2"):
            finished = True
        if not finished:
            phase_moe(l)
            if done(l, "moe"):
                finished = True
        if not finished:
            phase_final(l, act_tiles, route, last=(l == depth - 1))
            if done(l, "final"):
                finished = True
        P.release()
        if finished:
            break
    if dbg is not None:
        dump({"xres": xres, "zm": zm_d, "zg": zg_d, "abrT": abrT_d, "hT": hT_d, "yT": yT_d, "modd": modd, "h2": h2_d,
              "ys": ys_a, "tab": tab_d, "sb": sb_d}[dbg[0]])
    P.emit()
    st.close()
    return nc


def host_constants(cfg):
    import ml_dtypes
    C = 128
    out = {}
    out["c_ident"] = np.eye(128, dtype=np.float32)
    nf = 32
    inv = (np.float32(10000.0) ** (-np.arange(nf, dtype=np.float32) / np.float32(nf))).astype(np.float32)
    pos = np.arange(cfg.S)
    p_row = (pos // 64).astype(np.float32)
    p_col = (pos % 64).astype(np.float32)
    ang_r = (p_row[:, None] * inv[None, :]).astype(np.float32)
    ang_c = (p_col[:, None] * inv[None, :]).astype(np.float32)
    cosv = np.concatenate([np.cos(ang_r), np.cos(ang_c)], axis=1).astype(np.float32)
    sinv = np.concatenate([np.sin(ang_r), np.sin(ang_c)], axis=1).astype(np.float32)
    out["c_rope"] = np.concatenate([cosv, sinv], axis=1).reshape(cfg.NTL, 128, 128).astype(np.float32)
    j = np.arange(C, dtype=np.float32)[:, None]
    i = np.arange(C, dtype=np.float32)[None, :]
    rel = np.zeros((128, 4, 128), np.float32)
    rel[:, 0, :] = np.maximum(i - j, 0)
    rel[:, 1, :] = np.maximum(j - i, 0)
    rel[:, 2, :] = (i >= j)
    rel[:, 3, :] = (j > i)
    out["c_rel"] = rel
    idx = np.zeros((128, 4, 128), np.float32)
    idx[:, 0, :] = i + 1
    idx[:, 1, :] = C - i
    idx[:, 2, :] = C - 1 - j
    idx[:, 3, :] = j
    out["c_idx"] = idx
    tri = np.zeros((128, 2, 128), np.float32)
    tri[:, 0, :] = (j < i)
    tri[:, 1, :] = 1.0
    out["c_tri"] = tri
    out["c_ecap"] = np.tile((np.arange(32, dtype=np.float32) * cfg.CAP)[None, :], (128, 1)).astype(np.float32)
    tok = np.arange(cfg.NT * 128, dtype=np.int32).reshape(cfg.NT, 128, 1)
    out["c_tok"] = np.ascontiguousarray(np.tile(tok, (1, 1, 4)))
    out["c_tabinit"] = np.full((128, (cfg.NSLOT + 128) // 128 * 4), cfg.T, np.int32)
    out["c_zero"] = np.zeros((128, cfg.D), ml_dtypes.bfloat16)
    return out


def make_in_maps(cfg, inputs):
    x = np.asarray(inputs["x"], np.float32)
    B = x.shape[0]
    consts = host_constants(cfg)
    WT, NV, tile_var, tile_a, dr_idx, dc_idx, mask = na_variants(cfg)
    rpb = np.asarray(inputs["na_rpb"], np.float32)
    gathered = rpb[:, :, dr_idx, dc_idx]
    na_bias = np.where(mask[None, None] == 0.0, gathered, np.float32(NEG)).astype(np.float32)
    shared = {
        "ada_w": np.asarray(inputs["ada_w"], np.float32),
        "ada_b": np.asarray(inputs["ada_b"], np.float32),
        "norm1_g": np.asarray(inputs["norm1_g"], np.float32),
        "w_in": np.asarray(inputs["w_in"], np.float32),
        "gm_norm_g": np.asarray(inputs["gm_norm_g"], np.float32),
        "gm_ws": np.asarray(inputs["gm_ws"], np.float32),
        "gm_bsT": np.ascontiguousarray(np.asarray(inputs["gm_bs"], np.float32).transpose(0, 2, 1)),
        "na_bias": na_bias,
        "ret_decay_fwd": np.asarray(inputs["ret_decay_fwd"], np.float32),
        "ret_decay_bwd": np.asarray(inputs["ret_decay_bwd"], np.float32),
        "w_branch_a": np.asarray(inputs["w_branch_a"], np.float32),
        "w_branch_b": np.asarray(inputs["w_branch_b"], np.float32),
        "w_branch_c": np.asarray(inputs["w_branch_c"], np.float32),
        "w_out": np.asarray(inputs["w_out"], np.float32),
        "norm2_g": np.asarray(inputs["norm2_g"], np.float32),
        "w_router": np.ascontiguousarray(np.concatenate([np.asarray(inputs["moe_w_group"], np.float32),
                                                         np.asarray(inputs["moe_w_expert"], np.float32)], axis=-1)),
        "moe_w1": np.asarray(inputs["moe_w1"], np.float32),
        "moe_w3": np.asarray(inputs["moe_w3"], np.float32),
        "moe_w2": np.asarray(inputs["moe_w2"], np.float32),
        "final_norm_g": np.asarray(inputs["final_norm_g"], np.float32),
    }
    shared.update(consts)
    maps = []
    for b in range(B):
        m = dict(shared)
        m["x"] = np.ascontiguousarray(x[b])
        m["ctx"] = np.ascontiguousarray(np.asarray(inputs["ctx"], np.float32)[b])
        m["cvec"] = np.ascontiguousarray(np.stack([np.asarray(inputs["c"], np.float32)[b], np.asarray(inputs["c_ctx"], np.float32)]))
        maps.append(m)
    return maps


_NC_CACHE = {}


def run_cfg(cfg, inputs, stop_after=None, dbg=None, trace=False):
    key = (cfg.D, cfg.S, cfg.MH, cfg.CAP, stop_after, None if dbg is None else dbg[0])
    if key not in _NC_CACHE:
        _NC_CACHE[key] = build(cfg, stop_after=stop_after, dbg=dbg)
    nc = _NC_CACHE[key]
    maps = make_in_maps(cfg, inputs)
    res = run_bass_kernel_spmd(nc, maps, core_ids=list(range(len(maps))), **({"trace": True} if trace else {}))
    return res


def kernel(**inputs):
    cfg = Cfg()
    res = run_cfg(cfg, inputs)
    return np.stack([np.asarray(r["out"], np.float32) for r in res.results], axis=0)
```
